# Optimizing a Trainium2 kernel written in Bass

```python
import numpy as np
import jax
import jax.numpy as jnp
from jax import lax

D_MODEL = 1024
BATCH = 8
SEQ = 4096
DEPTH = 2

N_GROUPS = 4
HEAD_DIM = 64
GROUP_HEADS = D_MODEL // (N_GROUPS * HEAD_DIM)
GROUP_WIDTH = GROUP_HEADS * HEAD_DIM
MIX_WIDTH = N_GROUPS * GROUP_WIDTH
D_FF = 256 * ((8 * D_MODEL + 3 * 256 - 1) // (3 * 256))
N_ADA = 9
FFN_RESIDUAL_WEIGHT = 0.5
ROPE_THETA = 10000.0
RMS_EPS = 1e-6
NEG_INF = -1e30

MOBA_BLOCK = 256
MOBA_TOPK = 3
MOBA_QBLK = 32

MLA_Q_LORA = D_MODEL // 4
MLA_KV_LORA = D_MODEL // 8
MLA_NOPE = HEAD_DIM
MLA_ROPE = HEAD_DIM // 2
MLA_V = HEAD_DIM
MLA_QBLK = 128

NSA_CMP_LEN = 32
NSA_CMP_STRIDE = 16
NSA_CMP_HIDDEN = 4 * HEAD_DIM
NSA_SEL_BLOCK = 64
NSA_SEL_TOPN = 16
NSA_WINDOW = 512
NSA_FORCE_SCORE = 1e4
NSA_QBLK = 64

DSA_TOPK = 256
DSA_IDX_HEADS = 8
DSA_IDX_DIM = 32
DSA_QBLK = 64

IN_SIZES = (
    GROUP_WIDTH, GROUP_WIDTH, GROUP_WIDTH,
    MLA_Q_LORA, MLA_KV_LORA, MLA_ROPE,
    GROUP_WIDTH, HEAD_DIM, HEAD_DIM, HEAD_DIM, HEAD_DIM,
    HEAD_DIM, HEAD_DIM, 3 * GROUP_HEADS,
    GROUP_WIDTH, HEAD_DIM, HEAD_DIM,
    DSA_IDX_HEADS * DSA_IDX_DIM, DSA_IDX_DIM, DSA_IDX_HEADS,
)
N_IN = sum(IN_SIZES)

kernel_name = 'hybrid_moba_mla_nsa_dsa_macaron'


def rms_norm(x, g):
    x32 = x.astype(jnp.float32)
    y = x32 * lax.rsqrt(jnp.mean(x32 * x32, axis=-1, keepdims=True) + RMS_EPS)
    return (y * g.astype(jnp.float32)).astype(x.dtype)


def modulate(xn, shift, scale):
    return xn * (1 + scale) + shift


def swiglu(h, w_gate, w_up, w_down):
    return (jax.nn.silu(h @ w_gate) * (h @ w_up)) @ w_down


def rope_tables(n_pos, dim, dtype):
    inv_freq = 1.0 / (ROPE_THETA ** (np.arange(0, dim, 2, dtype=np.float32) / dim))
    ang = jnp.arange(n_pos, dtype=jnp.float32)[:, None] * jnp.asarray(inv_freq, jnp.float32)[None, :]
    return jnp.cos(ang).astype(dtype), jnp.sin(ang).astype(dtype)


def apply_rope(x, cos, sin):
    x1, x2 = jnp.split(x, 2, axis=-1)
    return jnp.concatenate([x1 * cos - x2 * sin, x2 * cos + x1 * sin], axis=-1)


def masked_softmax(s, mask):
    p = jax.nn.softmax(jnp.where(mask, s.astype(jnp.float32), NEG_INF), axis=-1)
    return jnp.where(mask, p, 0.0)


def map_query_blocks(fn, n_pos, qblk):
    starts = jnp.arange(n_pos // qblk, dtype=jnp.int32) * qblk
    out = lax.map(fn, starts)
    out = jnp.moveaxis(out, 0, 1)
    return out.reshape(out.shape[0], n_pos, *out.shape[3:])


def moba_attention(q, k, v, cos, sin):
    B, T, H, dh = q.shape
    q = apply_rope(q, cos[:, None], sin[:, None])
    k = apply_rope(k, cos[:, None], sin[:, None])
    nb = -(-T // MOBA_BLOCK)
    pad = nb * MOBA_BLOCK - T

    def to_blocks(t):
        t = jnp.pad(t, ((0, 0), (0, pad), (0, 0), (0, 0)))
        return t.reshape(B, nb, MOBA_BLOCK, H, dh).transpose(0, 3, 1, 2, 4)

    kb, vb = to_blocks(k), to_blocks(v)
    k_mean = jnp.mean(kb.astype(jnp.float32), axis=3).astype(q.dtype)
    topk = min(MOBA_TOPK, nb - 1)
    scale = dh ** -0.5
    b_ix = jnp.arange(B)[:, None, None, None]
    h_ix = jnp.arange(H)[None, :, None, None]
    blk_ids = jnp.arange(nb)
    offs = jnp.arange(MOBA_BLOCK)

    def block_fn(q0):
        qc = lax.dynamic_slice_in_dim(q, q0, MOBA_QBLK, axis=1)
        tq = q0 + jnp.arange(MOBA_QBLK)
        own = q0 // MOBA_BLOCK
        k_own = lax.dynamic_index_in_dim(kb, own, axis=2, keepdims=False)
        v_own = lax.dynamic_index_in_dim(vb, own, axis=2, keepdims=False)
        s_own = jnp.einsum('bqhd,bhkd->bhqk', qc, k_own) * scale
        m_own = jnp.broadcast_to((own * MOBA_BLOCK + offs)[None, :] <= tq[:, None], s_own.shape)
        if topk == 0:
            p = masked_softmax(s_own, m_own).astype(v.dtype)
            return jnp.einsum('bhqk,bhkd->bqhd', p, v_own)
        gate = jnp.einsum('bqhd,bhnd->bhqn', qc, k_mean).astype(jnp.float32)
        gate = jnp.where(blk_ids < own, gate, NEG_INF)
        _, idx = lax.top_k(gate, topk)
        k_sel = kb[b_ix, h_ix, idx]
        v_sel = vb[b_ix, h_ix, idx]
        s_sel = (jnp.einsum('bqhd,bhqnkd->bhqnk', qc, k_sel) * scale).reshape(B, H, MOBA_QBLK, topk * MOBA_BLOCK)
        m_sel = jnp.broadcast_to((idx < own)[..., None], (B, H, MOBA_QBLK, topk, MOBA_BLOCK)).reshape(s_sel.shape)
        p = masked_softmax(jnp.concatenate([s_sel, s_own], axis=-1),
                           jnp.concatenate([m_sel, m_own], axis=-1)).astype(v.dtype)
        p_sel = p[..., :topk * MOBA_BLOCK].reshape(B, H, MOBA_QBLK, topk, MOBA_BLOCK)
        p_own = p[..., topk * MOBA_BLOCK:]
        return (jnp.einsum('bhqnk,bhqnkd->bqhd', p_sel, v_sel)
                + jnp.einsum('bhqk,bhkd->bqhd', p_own, v_own))

    return map_query_blocks(block_fn, T, MOBA_QBLK)


def mla_attention(c_q, c_kv, k_rope, q_norm, w_uq, kv_norm, w_uk, w_uv, cos_r, sin_r):
    B, T, _ = c_q.shape
    H = GROUP_HEADS
    q = (rms_norm(c_q, q_norm) @ w_uq).reshape(B, T, H, MLA_NOPE + MLA_ROPE)
    q_nope = q[..., :MLA_NOPE]
    q_pe = apply_rope(q[..., MLA_NOPE:], cos_r[:, None], sin_r[:, None])
    ckv = rms_norm(c_kv, kv_norm)
    k_nope = (ckv @ w_uk).reshape(B, T, H, MLA_NOPE)
    v = (ckv @ w_uv).reshape(B, T, H, MLA_V)
    k_pe = apply_rope(k_rope, cos_r, sin_r)
    scale = (MLA_NOPE + MLA_ROPE) ** -0.5
    kpos = jnp.arange(T)

    def block_fn(q0):
        qn = lax.dynamic_slice_in_dim(q_nope, q0, MLA_QBLK, axis=1)
        qp = lax.dynamic_slice_in_dim(q_pe, q0, MLA_QBLK, axis=1)
        tq = q0 + jnp.arange(MLA_QBLK)
        s = (jnp.einsum('bqhd,bshd->bhqs', qn, k_nope)
             + jnp.einsum('bqhr,bsr->bhqs', qp, k_pe)) * scale
        p = masked_softmax(s, kpos[None, :] <= tq[:, None]).astype(v.dtype)
        return jnp.einsum('bhqs,bshd->bqhd', p, v)

    return map_query_blocks(block_fn, T, MLA_QBLK)


def nsa_attention(q, kc, vc, ks, vs, kw, vw, gate_logits, pe_k, pe_v,
                  cmp_k_w1, cmp_k_w2, cmp_v_w1, cmp_v_w2, cos, sin):
    B, T, H, dh = q.shape
    q = apply_rope(q, cos[:, None], sin[:, None])
    kc, ks, kw = (apply_rope(t, cos, sin) for t in (kc, ks, kw))
    n_cmp = (T - NSA_CMP_LEN) // NSA_CMP_STRIDE + 1
    cmp_start = np.arange(n_cmp) * NSA_CMP_STRIDE
    win_idx = cmp_start[:, None] + np.arange(NSA_CMP_LEN)[None, :]

    def compress(t, pe, w1, w2):
        blocks = (t[:, win_idx] + pe).reshape(B, n_cmp, NSA_CMP_LEN * dh)
        return jax.nn.silu(blocks @ w1) @ w2

    k_cmp = compress(kc, pe_k, cmp_k_w1, cmp_k_w2)
    v_cmp = compress(vc, pe_v, cmp_v_w1, cmp_v_w2)
    cmp_end = jnp.asarray(cmp_start + NSA_CMP_LEN - 1)
    n_sel = T // NSA_SEL_BLOCK
    sel_start = np.arange(n_sel) * NSA_SEL_BLOCK
    overlap = ((cmp_start[:, None] < sel_start[None, :] + NSA_SEL_BLOCK)
               & (cmp_start[:, None] + NSA_CMP_LEN > sel_start[None, :]))
    cmp_to_sel = jnp.asarray(overlap, jnp.float32)
    topn = min(NSA_SEL_TOPN, n_sel)
    ksb = ks.reshape(B, n_sel, NSA_SEL_BLOCK, dh)
    vsb = vs.reshape(B, n_sel, NSA_SEL_BLOCK, dh)
    kw_pad = jnp.pad(kw, ((0, 0), (NSA_WINDOW, 0), (0, 0)))
    vw_pad = jnp.pad(vw, ((0, 0), (NSA_WINDOW, 0), (0, 0)))
    gates = jax.nn.sigmoid(gate_logits.reshape(B, T, H, 3))
    scale = dh ** -0.5
    b_ix = jnp.arange(B)[:, None, None]
    sel_ids = jnp.arange(n_sel)
    sel_offs = jnp.arange(NSA_SEL_BLOCK)
    win_offs = jnp.arange(NSA_WINDOW + NSA_QBLK)

    def block_fn(q0):
        qc = lax.dynamic_slice_in_dim(q, q0, NSA_QBLK, axis=1)
        tq = q0 + jnp.arange(NSA_QBLK)
        s_c = jnp.einsum('bqhd,bnd->bhqn', qc, k_cmp) * scale
        p_c = masked_softmax(s_c, cmp_end[None, :] <= tq[:, None])
        o_c = jnp.einsum('bhqn,bnd->bqhd', p_c.astype(v_cmp.dtype), v_cmp)
        imp = jnp.einsum('bhqn,ns->bqs', p_c, cmp_to_sel)
        own = tq // NSA_SEL_BLOCK
        causal = sel_ids[None, :] <= own[:, None]
        forced = causal & ((sel_ids[None, :] == 0) | (sel_ids[None, :] >= own[:, None] - 1))
        imp = jnp.where(forced, NSA_FORCE_SCORE, jnp.where(causal, imp, -NSA_FORCE_SCORE))
        _, idx = lax.top_k(imp, topn)
        k_sel = ksb[b_ix, idx]
        v_sel = vsb[b_ix, idx]
        s_s = (jnp.einsum('bqhd,bqnkd->bhqnk', qc, k_sel) * scale).reshape(B, H, NSA_QBLK, topn * NSA_SEL_BLOCK)
        pos = idx[..., None] * NSA_SEL_BLOCK + sel_offs
        m_s = (pos <= tq[None, :, None, None]).reshape(B, 1, NSA_QBLK, topn * NSA_SEL_BLOCK)
        p_s = masked_softmax(s_s, m_s).astype(v_sel.dtype).reshape(B, H, NSA_QBLK, topn, NSA_SEL_BLOCK)
        o_s = jnp.einsum('bhqnk,bqnkd->bqhd', p_s, v_sel)
        kwc = lax.dynamic_slice_in_dim(kw_pad, q0, NSA_WINDOW + NSA_QBLK, axis=1)
        vwc = lax.dynamic_slice_in_dim(vw_pad, q0, NSA_WINDOW + NSA_QBLK, axis=1)
        kpos = q0 - NSA_WINDOW + win_offs
        m_w = ((kpos[None, :] <= tq[:, None]) & (kpos[None, :] > tq[:, None] - NSA_WINDOW)
               & (kpos[None, :] >= 0))
        s_w = jnp.einsum('bqhd,bkd->bhqk', qc, kwc) * scale
        p_w = masked_softmax(s_w, m_w).astype(vwc.dtype)
        o_w = jnp.einsum('bhqk,bkd->bqhd', p_w, vwc)
        g = lax.dynamic_slice_in_dim(gates, q0, NSA_QBLK, axis=1)
        return g[..., 0:1] * o_c + g[..., 1:2] * o_s + g[..., 2:3] * o_w

    return map_query_blocks(block_fn, T, NSA_QBLK)


def dsa_attention(q, k, v, iq, ik, iw, cos, sin):
    B, T, H, dh = q.shape
    q = apply_rope(q, cos[:, None], sin[:, None])
    k = apply_rope(k, cos, sin)
    iq = iq.reshape(B, T, DSA_IDX_HEADS, DSA_IDX_DIM)
    topk = min(DSA_TOPK, T // 4)
    idx_scale = (DSA_IDX_HEADS * DSA_IDX_DIM) ** -0.5
    scale = dh ** -0.5
    b_ix = jnp.arange(B)[:, None, None]
    kpos = jnp.arange(T)

    def block_fn(q0):
        qc = lax.dynamic_slice_in_dim(q, q0, DSA_QBLK, axis=1)
        iqc = lax.dynamic_slice_in_dim(iq, q0, DSA_QBLK, axis=1)
        iwc = lax.dynamic_slice_in_dim(iw, q0, DSA_QBLK, axis=1)
        tq = q0 + jnp.arange(DSA_QBLK)
        score = jnp.einsum('bqh,bqhs->bqs', iwc,
                           jax.nn.relu(jnp.einsum('bqhd,bsd->bqhs', iqc, ik))).astype(jnp.float32) * idx_scale
        score = jnp.where(kpos[None, None, :] <= tq[None, :, None], score, NEG_INF)
        _, idx = lax.top_k(score, topk)
        k_sel = k[b_ix, idx]
        v_sel = v[b_ix, idx]
        s = jnp.einsum('bqhd,bqkd->bhqk', qc, k_sel) * scale
        p = masked_softmax(s, (idx <= tq[None, :, None])[:, None]).astype(v_sel.dtype)
        return jnp.einsum('bhqk,bqkd->bqhd', p, v_sel)

    return map_query_blocks(block_fn, T, DSA_QBLK)


def hybrid_mixer(h, w_in, mla_q_norm, mla_w_uq, mla_kv_norm, mla_w_uk, mla_w_uv,
                 nsa_pe_k, nsa_pe_v, nsa_cmp_k_w1, nsa_cmp_k_w2, nsa_cmp_v_w1, nsa_cmp_v_w2,
                 group_norm, w_out, cos, sin, cos_r, sin_r):
    B, T, _ = h.shape
    splits = np.cumsum(IN_SIZES)[:-1].tolist()
    (mq, mk, mv, cq, ckv, kr, nq, nkc, nvc, nks, nvs, nkw, nvw, ngate,
     dq, dk, dv, diq, dik, diw) = jnp.split(h @ w_in, splits, axis=-1)

    def heads(t):
        return t.reshape(B, T, GROUP_HEADS, HEAD_DIM)

    o_moba = moba_attention(heads(mq), heads(mk), heads(mv), cos, sin)
    o_mla = mla_attention(cq, ckv, kr, mla_q_norm, mla_w_uq, mla_kv_norm, mla_w_uk, mla_w_uv, cos_r, sin_r)
    o_nsa = nsa_attention(heads(nq), nkc, nvc, nks, nvs, nkw, nvw, ngate, nsa_pe_k, nsa_pe_v,
                          nsa_cmp_k_w1, nsa_cmp_k_w2, nsa_cmp_v_w1, nsa_cmp_v_w2, cos, sin)
    o_dsa = dsa_attention(heads(dq), dk, dv, diq, dik, diw, cos, sin)
    groups = [o.reshape(B, T, GROUP_WIDTH) for o in (o_moba, o_mla, o_nsa, o_dsa)]
    y = jnp.concatenate([rms_norm(o, group_norm[i]) for i, o in enumerate(groups)], axis=-1)
    return y @ w_out


def setup_inputs(seed: int = 0) -> dict:
    key = jax.random.key(seed)
    keys = list(jax.random.split(key, 32))

    def nrm(shape, scale):
        return jax.random.normal(keys.pop(), shape, jnp.float32) * scale

    def gain(shape):
        return 1.0 + nrm(shape, 0.05)

    L = DEPTH
    cmp_in = NSA_CMP_LEN * HEAD_DIM
    return {
        'x': nrm((BATCH, SEQ, D_MODEL), 1.0),
        'c': nrm((BATCH, D_MODEL), 1.0),
        'ada_w': nrm((L, D_MODEL, N_ADA * D_MODEL), 0.5 * D_MODEL ** -0.5),
        'ada_b': nrm((L, N_ADA * D_MODEL), 0.02),
        'ffn1_norm': gain((L, D_MODEL)),
        'ffn1_w_gate': nrm((L, D_MODEL, D_FF), D_MODEL ** -0.5),
        'ffn1_w_up': nrm((L, D_MODEL, D_FF), D_MODEL ** -0.5),
        'ffn1_w_down': nrm((L, D_FF, D_MODEL), D_FF ** -0.5),
        'mix_norm': gain((L, D_MODEL)),
        'w_in': nrm((L, D_MODEL, N_IN), D_MODEL ** -0.5),
        'mla_q_norm': gain((L, MLA_Q_LORA)),
        'mla_w_uq': nrm((L, MLA_Q_LORA, GROUP_HEADS * (MLA_NOPE + MLA_ROPE)), MLA_Q_LORA ** -0.5),
        'mla_kv_norm': gain((L, MLA_KV_LORA)),
        'mla_w_uk': nrm((L, MLA_KV_LORA, GROUP_HEADS * MLA_NOPE), MLA_KV_LORA ** -0.5),
        'mla_w_uv': nrm((L, MLA_KV_LORA, GROUP_HEADS * MLA_V), MLA_KV_LORA ** -0.5),
        'nsa_pe_k': nrm((L, NSA_CMP_LEN, HEAD_DIM), 0.5),
        'nsa_pe_v': nrm((L, NSA_CMP_LEN, HEAD_DIM), 0.5),
        'nsa_cmp_k_w1': nrm((L, cmp_in, NSA_CMP_HIDDEN), cmp_in ** -0.5),
        'nsa_cmp_k_w2': nrm((L, NSA_CMP_HIDDEN, HEAD_DIM), NSA_CMP_HIDDEN ** -0.5),
        'nsa_cmp_v_w1': nrm((L, cmp_in, NSA_CMP_HIDDEN), cmp_in ** -0.5),
        'nsa_cmp_v_w2': nrm((L, NSA_CMP_HIDDEN, HEAD_DIM), NSA_CMP_HIDDEN ** -0.5),
        'group_norm': gain((L, N_GROUPS, GROUP_WIDTH)),
        'w_out': nrm((L, MIX_WIDTH, D_MODEL), MIX_WIDTH ** -0.5),
        'ffn2_norm': gain((L, D_MODEL)),
        'ffn2_w_gate': nrm((L, D_MODEL, D_FF), D_MODEL ** -0.5),
        'ffn2_w_up': nrm((L, D_MODEL, D_FF), D_MODEL ** -0.5),
        'ffn2_w_down': nrm((L, D_FF, D_MODEL), D_FF ** -0.5),
        'final_norm': gain((D_MODEL,)),
    }


def reference(x, c, ada_w, ada_b, ffn1_norm, ffn1_w_gate, ffn1_w_up, ffn1_w_down, mix_norm, w_in,
              mla_q_norm, mla_w_uq, mla_kv_norm, mla_w_uk, mla_w_uv,
              nsa_pe_k, nsa_pe_v, nsa_cmp_k_w1, nsa_cmp_k_w2, nsa_cmp_v_w1, nsa_cmp_v_w2,
              group_norm, w_out, ffn2_norm, ffn2_w_gate, ffn2_w_up, ffn2_w_down, final_norm):
    B, T, D = x.shape
    cos, sin = rope_tables(T, HEAD_DIM, x.dtype)
    cos_r, sin_r = rope_tables(T, MLA_ROPE, x.dtype)
    c_act = jax.nn.silu(c)
    for l in range(DEPTH):
        sh1, sc1, g1, sh2, sc2, g2, sh3, sc3, g3 = jnp.split(
            (c_act @ ada_w[l] + ada_b[l])[:, None, :], N_ADA, axis=-1)
        h = modulate(rms_norm(x, ffn1_norm[l]), sh1, sc1)
        x = x + FFN_RESIDUAL_WEIGHT * g1 * swiglu(h, ffn1_w_gate[l], ffn1_w_up[l], ffn1_w_down[l])
        h = modulate(rms_norm(x, mix_norm[l]), sh2, sc2)
        x = x + g2 * hybrid_mixer(h, w_in[l], mla_q_norm[l], mla_w_uq[l], mla_kv_norm[l], mla_w_uk[l], mla_w_uv[l],
                                  nsa_pe_k[l], nsa_pe_v[l], nsa_cmp_k_w1[l], nsa_cmp_k_w2[l],
                                  nsa_cmp_v_w1[l], nsa_cmp_v_w2[l], group_norm[l], w_out[l],
                                  cos, sin, cos_r, sin_r)
        h = modulate(rms_norm(x, ffn2_norm[l]), sh3, sc3)
        x = x + FFN_RESIDUAL_WEIGHT * g3 * swiglu(h, ffn2_w_gate[l], ffn2_w_up[l], ffn2_w_down[l])
    return rms_norm(x, final_norm)
```

```python
import numpy as np
import concourse.bass as bass
import concourse.mybir as mybir
from concourse.bass_utils import run_bass_kernel_spmd

F32 = mybir.dt.float32
BF16 = mybir.dt.bfloat16
AF = mybir.ActivationFunctionType
ALU = mybir.AluOpType
AX = mybir.AxisListType

ENGS = ['pe', 'act', 'dve', 'pool', 'sp']
SEG = 16000
T = 4096
D = 1024
DFF = 2816
NCH = 8
NEGB = -30000.0
NPSB = 7


class Res:
    __slots__ = ('name', 'wr', 'rd', 'dsem', 'dcnt', 'psum')

    def __init__(self, name):
        self.name = name
        self.psum = False
        self.wr = {}
        self.rd = {}
        self.dsem = None
        self.dcnt = 0


class TT:
    def __init__(self, t, name, nslots=1):
        self.t = t
        self.name = name
        self.r = [Res("%s.%d" % (name, i)) for i in range(nslots)]

    def __getitem__(self, idx):
        return self.t[idx]


class Sched:
    def __init__(self, nc):
        self.nc = nc
        self.q = {e: [] for e in ENGS}
        self.cnt = {e: 0 for e in ENGS}
        self.waited = {e: {} for e in ENGS}
        self.sems = {}
        self.semctx = []
        self.out_events = {}
        self.dma_tot = {}
        self.uid = 0
        self.sb_base = 16640
        self.sb_off = self.sb_base
        self.sb_lim = 229376 - 64
        self.sb_peak = 0
        self.live = []
        self.free_dsems = []

    def tile(self, shape, dtype, name, nslots=1):
        esz = 2 if dtype == BF16 else 4
        n = 1
        for s in shape[1:]:
            n *= s
        size = (n * esz + 63) // 64 * 64
        self.uid += 1
        t = self.nc.alloc_sbuf_tensor_at("%s_%d" % (name, self.uid), list(shape), dtype, offset=self.sb_off)
        self.sb_off += size
        assert self.sb_off <= self.sb_lim, ("SBUF overflow", name, self.sb_off)
        self.sb_peak = max(self.sb_peak, self.sb_off)
        tt_ = TT(t, name, nslots)
        self.live.append((self.sb_off - size, tt_))
        return tt_

    def mark(self):
        return self.sb_off

    def release(self, m):
        self.barrier()
        self.sb_off = m
        keep = []
        for off, tt_ in self.live:
            if off >= m:
                for r in tt_.r:
                    if r.dsem is not None:
                        self.free_dsems.append((r.dsem, r.dcnt))
                        r.dsem = None
            else:
                keep.append((off, tt_))
        self.live = keep

    def sem(self, key):
        s = self.sems.get(key)
        if s is None:
            ctx = self.nc.semaphore("s%d" % len(self.sems))
            s = ctx.__enter__()
            self.semctx.append(ctx)
            self.sems[key] = s
        return s

    def _collect(self, eng, reads, writes, partial):
        deps = {}
        for r in reads:
            for k, v in r.wr.items():
                if deps.get(k, 0) < v:
                    deps[k] = v
            if r.psum:
                for k, v in r.rd.items():
                    if k[0] != eng and deps.get(k, 0) < v:
                        deps[k] = v
        for w in writes:
            if not partial:
                for k, v in w.wr.items():
                    if deps.get(k, 0) < v:
                        deps[k] = v
            for k, v in w.rd.items():
                if deps.get(k, 0) < v:
                    deps[k] = v
        wl = []
        wd = self.waited[eng]
        for k, v in deps.items():
            if eng == 'pe' and k[0] == 'pe':
                continue
            if wd.get(k, 0) < v:
                wd[k] = v
                wl.append((self.sem(k), v))
        return wl

    def op(self, eng, fn, reads=(), writes=(), partial=False):
        wl = self._collect(eng, reads, writes, partial)
        self.cnt[eng] += 1
        n = self.cnt[eng]
        key = (eng, (n - 1) // SEG)
        val = (n - 1) % SEG + 1
        self.q[eng].append((wl, fn, (self.sem(key), 1)))
        for r in reads:
            if r.rd.get(key, 0) < val:
                r.rd[key] = val
        for w in writes:
            if w.wr.get(key, 0) < val:
                w.wr[key] = val

    def dma(self, eng, out, in_, reads=(), writes=(), partial=True, is_output=False, **kw):
        wl = self._collect(eng, reads, writes, partial)
        w0 = writes[0]
        if w0.dsem is None:
            if self.free_dsems:
                w0.dsem, w0.dcnt = self.free_dsems.pop()
                if w0.dcnt > 40000:
                    self.uid += 1
                    w0.dsem, w0.dcnt = ('dma', self.uid), 0
            else:
                self.uid += 1
                w0.dsem, w0.dcnt = ('dma', self.uid), 0
        w0.dcnt += 16
        key = w0.dsem
        val = w0.dcnt
        assert val < 60000, w0.name
        self.dma_tot[key] = val

        def fn(e, out=out, in_=in_, kw=kw):
            return e.dma_start(out=out, in_=in_, **kw)
        self.q[eng].append((wl, fn, (self.sem(key), 16)))
        for r in reads:
            if r.rd.get(key, 0) < val:
                r.rd[key] = val
        for w in writes:
            if w.wr.get(key, 0) < val:
                w.wr[key] = val
        if is_output:
            self.out_events[key] = val

    def barrier(self):
        evs = {}
        for e in ENGS:
            n = self.cnt[e]
            if n:
                evs[(e, (n - 1) // SEG)] = (n - 1) % SEG + 1
        evs.update(self.dma_tot)
        for e in ENGS:
            wl = []
            wd = self.waited[e]
            for k, v in evs.items():
                if e == 'pe' and k[0] == 'pe':
                    continue
                if wd.get(k, 0) < v:
                    wd[k] = v
                    wl.append((self.sem(k), v))
            if wl:
                self.q[e].append((wl, None, None))

    def finish(self):
        nc = self.nc
        self.barrier()
        q = self.q
        with nc.Block() as block:
            def replay(name):
                def run(eng):
                    for waits, fn, inc in q[name]:
                        for s, v in waits:
                            eng.wait_ge(s, v)
                        if fn is not None:
                            ins = fn(eng)
                            if inc is not None:
                                ins.then_inc(inc[0], inc[1])
                return run
            block.tensor(replay('pe'))
            block.scalar(replay('act'))
            block.vector(replay('dve'))
            block.gpsimd(replay('pool'))
            block.sync(replay('sp'))
        for ctx in reversed(self.semctx):
            ctx.__exit__(None, None, None)

    def mm(self, out, lhsT, rhs, start, stop, R, W):
        self.op('pe', lambda e: e.matmul(out, lhsT=lhsT, rhs=rhs, start=start, stop=stop), R, W)

    def tr(self, out, in_, ident, R, W):
        self.op('pe', lambda e: e.transpose(out, in_, ident), R, W)

    def act(self, out, in_, func, R, W, **kw):
        self.op('act', lambda e: e.activation(out=out, in_=in_, func=func, **kw), R, W)

    def cp(self, eng, out, in_, R, W, partial=False):
        if eng == 'act':
            self.op('act', lambda e: e.copy(out=out, in_=in_), R, W, partial)
        else:
            self.op(eng, lambda e: e.tensor_copy(out=out, in_=in_), R, W, partial)

    def tt(self, eng, out, in0, in1, op, R, W):
        self.op(eng, lambda e: e.tensor_tensor(out=out, in0=in0, in1=in1, op=op), R, W)

    def ts(self, eng, out, in0, s1, s2, op0, op1, R, W, accum_out=None):
        if accum_out is not None:
            self.op(eng, lambda e: e.tensor_scalar(out=out, in0=in0, scalar1=s1, scalar2=s2, op0=op0, op1=op1,
                                                   accum_out=accum_out), R, W)
        elif op1 is None:
            self.op(eng, lambda e: e.tensor_scalar(out=out, in0=in0, scalar1=s1, scalar2=None, op0=op0), R, W)
        else:
            self.op(eng, lambda e: e.tensor_scalar(out=out, in0=in0, scalar1=s1, scalar2=s2, op0=op0, op1=op1), R, W)

    def stt(self, out, in0, scalar, in1, op0, op1, R, W):
        self.op('dve', lambda e: e.scalar_tensor_tensor(out=out, in0=in0, scalar=scalar, in1=in1, op0=op0, op1=op1),
                R, W)

    def memset(self, eng, ap, val, W):
        self.op(eng, lambda e: e.memset(ap, val), (), W)


IN_SIZES = (256, 256, 256, 256, 128, 32, 256, 64, 64, 64, 64, 64, 64, 12, 256, 64, 64, 256, 32, 8)
IN_OFF = np.concatenate([[0], np.cumsum(IN_SIZES)]).astype(int)
(I_MQ, I_MK, I_MV, I_CQ, I_CKV, I_KR, I_NQ, I_NKC, I_NVC, I_NKS, I_NVS, I_NKW, I_NVW, I_NG,
 I_DQ, I_DK, I_DV, I_DIQ, I_DIK, I_DIW) = range(20)


def _cols(i):
    return np.arange(IN_OFF[i], IN_OFF[i + 1])


def _swap(c, half):
    c = np.asarray(c).reshape(-1, 2, half)
    return c[:, ::-1, :].reshape(-1)


def win_layout():
    segs = {}
    idx = []

    def add(name, cols):
        segs[name] = (len(idx), len(cols))
        idx.extend(list(cols))

    def addr(nm, i):
        c = _cols(i)
        add(nm, c)
        add(nm + '_s', _swap(c, 32))
    segs['MOBA0'] = (len(idx), 0)
    addr('mq', I_MQ)
    addr('mk', I_MK)
    add('mv', _cols(I_MV))
    segs['MOBA1'] = (len(idx), 0)
    segs['MLA0'] = (len(idx), 0)
    add('cq', _cols(I_CQ))
    add('ckv', _cols(I_CKV))
    kr = _cols(I_KR)
    add('kr96', np.concatenate([_cols(I_CKV)[:64], kr]))
    add('kr96_s', np.concatenate([_cols(I_CKV)[:64], _swap(kr, 16)]))
    segs['MLA1'] = (len(idx), 0)
    segs['NSA0'] = (len(idx), 0)
    addr('nq', I_NQ)
    addr('nkc', I_NKC)
    addr('nks', I_NKS)
    addr('nkw', I_NKW)
    add('nvc', _cols(I_NVC))
    add('nvs', _cols(I_NVS))
    add('nvw', _cols(I_NVW))
    add('ngrep', np.repeat(_cols(I_NG), 64))
    segs['NSA1'] = (len(idx), 0)
    segs['DSA0'] = (len(idx), 0)
    addr('dq', I_DQ)
    addr('dk', I_DK)
    add('dv', _cols(I_DV))
    iq = _cols(I_DIQ)
    iqc = []
    for ch in range(3):
        for k in range(4):
            hh = 3 * ch + k
            iqc.append(iq[hh * 32:(hh + 1) * 32] if (k < 3 and hh < 8) else iq[0:32])
    add('diq', np.concatenate(iqc))
    add('dik4', np.tile(_cols(I_DIK), 4))
    add('diw', _cols(I_DIW))
    segs['DSA1'] = (len(idx), 0)
    return np.asarray(idx, dtype=np.int64), segs


WIN_IDX, WSEG = win_layout()
NWIN = len(WIN_IDX)


def rope_tabs():
    def tabs(dim):
        inv = 1.0 / (10000.0 ** (np.arange(0, dim, 2, dtype=np.float32) / dim))
        ang = np.arange(T, dtype=np.float32)[:, None] * inv[None, :].astype(np.float32)
        return np.cos(ang).astype(np.float32), np.sin(ang).astype(np.float32)
    c64, s64 = tabs(64)
    c32, s32 = tabs(32)
    C = np.zeros((128, T), np.float32)
    Sg = np.zeros((128, T), np.float32)
    for b in (0, 64):
        C[b:b + 32] = c64.T
        C[b + 32:b + 64] = c64.T
        Sg[b:b + 32] = -s64.T
        Sg[b + 32:b + 64] = s64.T
    Cm = np.ones((128, T), np.float32)
    Sm = np.zeros((128, T), np.float32)
    Cm[64:80] = c32.T
    Cm[80:96] = c32.T
    Sm[64:80] = -s32.T
    Sm[80:96] = s32.T
    return C, Sg, Cm, Sm


def const_tables():
    import ml_dtypes
    bf = ml_dtypes.bfloat16
    k = np.arange(128)[:, None]
    q = np.arange(512)[None, :]
    mdiag = np.stack([((i * 128 + k) <= q) for i in range(4)]).astype(np.float32)
    mwin = np.stack([((r * 128 + k) > (q - 512)) for r in (-4, -3, -2, -1)]).astype(np.float32)
    masks = np.concatenate([mdiag, mwin], 0).transpose(1, 0, 2).astype(bf)
    key = np.arange(T)[None, :]
    e16 = (key // 256 == np.arange(16)[:, None]).astype(np.float32)
    e64 = (key // 64 == np.arange(64)[:, None]).astype(np.float32)
    eind = np.zeros((128, 2, T), np.float32)
    eind[64:80, 0] = e16
    eind[64:128, 1] = e64
    eind = eind.astype(bf)
    n = np.arange(256)[:, None]
    mc = ((16 * n + 31) <= np.arange(T)[None, :]) & (n < 255)
    mcmp = mc.reshape(2, 128, T).transpose(1, 0, 2).astype(bf)
    cs = np.arange(255) * 16
    ss = np.arange(64) * 64
    ov = ((cs[:, None] < ss[None, :] + 64) & (cs[:, None] + 32 > ss[None, :])).astype(np.float32)
    c2s = np.zeros((256, 65), np.float32)
    c2s[:255, :64] = ov
    c2s[:255, 64] = 1.0
    c2s = c2s.reshape(2, 128, 65).transpose(1, 0, 2).astype(bf)
    tq = np.arange(T)[:, None]
    sid = np.arange(64)[None, :]
    own = tq // 64
    causal = sid <= own
    forced = causal & ((sid == 0) | (sid >= own - 1))
    A = (causal & ~forced).astype(np.float32)
    Bm = np.where(forced, 1e4, np.where(causal, 0.0, -1e4)).astype(np.float32)
    A = A.reshape(32, 128, 64).transpose(1, 0, 2).copy()
    Bm = Bm.reshape(32, 128, 64).transpose(1, 0, 2).copy()
    qq = np.arange(128)[:, None]
    kk = np.arange(128)[None, :]
    dbias = np.where(kk <= qq, 0.0, -1e30).astype(np.float32)
    d01 = (kk <= qq).astype(np.float32).astype(bf)
    ident = np.eye(128, dtype=np.float32)
    return dict(masks=masks, eind=eind, mcmp=mcmp, c2s=c2s, nsaA=A, nsaB=Bm, dbias=dbias, d01=d01,
                ident=ident, identb=ident.astype(bf))


def pcol(v, n):
    return np.ascontiguousarray(np.asarray(v, np.float32).reshape(n, 128).T)


class K:
    pass


def build(nlayers=2, dbg=(), stop=None):
    nc = bass.Bass("TRN2", target_bir_lowering=False)
    S = Sched(nc)
    g = K()
    g.nc, g.S, g.dbg, g.stop = nc, S, set(dbg), stop
    din = {}

    def inp(name, shape, dt=F32):
        din[name] = nc.dram_tensor(name, list(shape), dt, kind="ExternalInput").ap()
        return din[name]
    g.din = din
    L = 2
    inp('x', [T, D])
    inp('cT', [128, 8])
    inp('ada_w', [L, D, 9 * D])
    inp('ada_bT', [L, 128, 72])
    inp('normsT', [L, 128, 3, 8])
    inp('fnorm_b', [128, D])
    for nm in ('ffn1', 'ffn2'):
        inp(nm + '_w_gate', [L, D, DFF])
        inp(nm + '_w_up', [L, D, DFF])
        inp(nm + '_w_down', [L, DFF, D])
    inp('w_in_g', [L, D, NWIN])
    inp('w_out', [L, D, D])
    inp('gnT', [L, 128, 8])
    inp('mla_qnT', [L, 128, 2])
    inp('mla_kvnT', [L, 128, 1])
    inp('mla_w_uq', [L, 256, 384])
    inp('mla_w_uq_s', [L, 256, 384])
    inp('mla_w_uk', [L, 128, 256])
    inp('mla_w_uv', [L, 128, 256])
    inp('nsa_peT', [L, 64, 2, 32])
    inp('nsa_w1', [L, 2, 2048, 256])
    inp('nsa_w2', [L, 2, 256, 64])
    inp('ropeC', [128, T])
    inp('ropeS', [128, T])
    inp('ropeCm', [128, T])
    inp('ropeSm', [128, T])
    inp('masks', [128, 8, 512], BF16)
    inp('eind', [128, 2, T], BF16)
    inp('mcmp', [128, 2, T], BF16)
    inp('c2s', [128, 2, 65], BF16)
    inp('nsaA', [128, 32, 64])
    inp('nsaB', [128, 32, 64])
    inp('dbias', [128, 128])
    inp('d01', [128, 128], BF16)
    inp('ident', [128, 128])
    inp('identb', [128, 128], BF16)

    def scratch(name, shape, dt):
        kind = "ExternalOutput" if name in g.dbg else "Internal"
        return nc.dram_tensor(name, list(shape), dt, kind=kind).ap()
    g.out = nc.dram_tensor("out", [T, D], F32, kind="ExternalOutput").ap()
    g.XT = scratch('XT', [8, 128, T], F32)
    g.HT = scratch('HT', [8, 128, T], BF16)
    g.OT = scratch('OT', [8, 128, T], F32)
    g.XTr = [Res('XT%d' % i) for i in range(NCH)]
    g.HTr = [Res('HT%d' % i) for i in range(NCH)]
    g.OTr = [[Res('OT%d_%d' % (gi, i)) for i in range(NCH)] for gi in range(4)]
    g.OUTr = [Res('out%d' % i) for i in range(NCH)]
    g.noR = []

    g.ps = [TT(nc.alloc_psum_tensor("psb%d" % i, [128, 512], F32), "psb%d" % i) for i in range(7)]
    for b_ in g.ps:
        b_.r[0].psum = True
    g.psi = 0
    g.dsa_nc = globals().get('DSA_NC', 1)
    g.pso = 0

    g.ident = S.tile([128, 128], F32, 'ident')
    g.identb = S.tile([128, 128], BF16, 'identb')
    g.onesb = S.tile([128, 128], BF16, 'onesb')
    g.sel64 = S.tile([128, 64], F32, 'sel64')
    g.ada = [S.tile([128, 72], F32, 'ada%d' % l) for l in range(L)]
    g.norms = [S.tile([128, 3, 8], F32, 'norms%d' % l) for l in range(L)]
    g.modAs = [[S.tile([128, 8], F32, 'modA%d_%d' % (l, i)) for i in range(3)] for l in range(L)]
    g.modGs = [[S.tile([128, 8], F32, 'modG%d_%d' % (l, i)) for i in range(3)] for l in range(L)]
    g.norm_done = set()
    S.dma('sp', g.ident[:], din['ident'], writes=g.ident.r)
    S.dma('sp', g.identb[:], din['identb'], writes=g.identb.r)
    S.memset('pool', g.onesb[:], 1.0, g.onesb.r)
    S.memset('pool', g.sel64[:], 0.0, g.sel64.r)
    S.memset('pool', g.sel64[64:65, :], 1.0, g.sel64.r)
    for l in range(L):
        S.dma('sp', g.norms[l][:], din['normsT'][l], writes=g.norms[l].r)

    prologue(g)
    for l in range(nlayers):
        adaln(g, l)
        for sub in range(3):
            set_mod(g, l, sub)
    if stop == 'pro':
        if 'ADA0' in g.dbg:
            dd = nc.dram_tensor('ADA0', [128, 72], F32, kind='ExternalOutput').ap()
            S.dma('sp', dd, g.ada[0][:], reads=g.ada[0].r, writes=[Res('ada0dbg')])
        S.finish()
        return nc
    for l in range(nlayers):
        if 'skipffn' not in g.dbg:
            ffn(g, l, 0, next_norm=(l, 1))
        if stop in ('ffn1', 'norm'):
            break
        mixer(g, l)
        if stop in ('mix', 'moba', 'mla', 'nsa', 'dsa'):
            break
        ffn(g, l, 2, next_norm=((l + 1, 0) if l + 1 < nlayers else None))
    if stop is None:
        epilogue(g)
    S.finish()
    print("[build] instr counts", S.cnt, "sems", len(S.sems), "sbuf peak", S.sb_peak)
    return nc


def dump(g, name, ap, shape, R, dt=F32):
    if name in g.dbg:
        dd = g.nc.dram_tensor(name, list(shape), dt, kind='ExternalOutput').ap()
        g.S.dma('sp', dd, ap, reads=R, writes=[Res(name + 'dbg')])


def psum(g):
    b = g.ps[g.psi % 5]
    g.psi += 1
    return b


def psum_o(g):
    b = g.ps[5 + g.pso % 2]
    g.pso += 1
    return b


def prologue(g):
    S, din = g.S, g.din
    m = S.mark()
    xs = [S.tile([128, D], F32, 'pxs%d' % i) for i in range(2)]
    xc = [S.tile([128, 8, 512], F32, 'pxc%d' % i) for i in range(2)]
    for tc in range(NCH):
        xct = xc[tc % 2]
        for j in range(4):
            tt_ = tc * 4 + j
            xt = xs[tt_ % 2]
            S.dma('sp', xt[:], din['x'][tt_ * 128:(tt_ + 1) * 128, :], writes=xt.r)
            for hb in range(2):
                pb = psum(g)
                for dd in range(4):
                    dc = hb * 4 + dd
                    S.tr(pb[:, dd * 128:(dd + 1) * 128], xt[:, dc * 128:(dc + 1) * 128], g.ident[:],
                         [xt.r[0], g.ident.r[0]], pb.r)
                eng = 'act' if hb == 0 else 'dve'
                S.cp(eng, xct[:, hb * 4:(hb + 1) * 4, j * 128:(j + 1) * 128],
                     pb[:].rearrange("p (a b) -> p a b", a=4), pb.r, xct.r, partial=True)
        S.dma('sp', g.XT[:, :, tc * 512:(tc + 1) * 512].rearrange("c p t -> p c t"), xct[:],
              reads=xct.r, writes=[g.XTr[tc]], partial=False)
    S.release(m)


def eng_is_act(e):
    return hasattr(e, 'activation')


def adaln(g, l):
    S, din = g.S, g.din
    m = S.mark()
    cT = S.tile([128, 8], F32, 'cT')
    cact = S.tile([128, 8], F32, 'cact')
    arow = S.tile([1, 9 * D], F32, 'arow')
    one1 = S.tile([1, 1], F32, 'one1')
    bT = S.tile([128, 72], F32, 'adab')
    S.dma('sp', cT[:], din['cT'], writes=cT.r)
    S.dma('sp', bT[:], din['ada_bT'][l], writes=bT.r)
    S.memset('pool', one1[:], 1.0, one1.r)
    S.act(cact[:], cT[:], AF.Silu, cT.r, cact.r)
    NB = 1152
    wb = [S.tile([128, 8, NB], F32, 'adaw%d' % i) for i in range(2)]
    for blk in range(8):
        w = wb[blk % 2]
        S.dma('sp' if blk % 2 == 0 else 'act', w[:],
              din['ada_w'][l][:, blk * NB:(blk + 1) * NB].rearrange("(c p) n -> p c n", p=128),
              writes=w.r, partial=False)
        for sub in range(3):
            n0 = sub * 384
            pb = psum(g)
            for dc in range(8):
                S.mm(pb[0:1, 0:384], cact[:, dc:dc + 1], w[:, dc, n0:n0 + 384], dc == 0, dc == 7,
                     [cact.r[0], w.r[0]], pb.r)
            S.cp('act', arow[0:1, blk * NB + n0: blk * NB + n0 + 384], pb[0:1, 0:384], pb.r, arow.r)
    dump(g, 'AROW%d' % l, arow[:], [1, 9 * D], arow.r)
    dump(g, 'CACT%d' % l, cact[:], [128, 8], cact.r)
    pb = psum(g)
    for j in range(72):
        S.mm(pb[:, j:j + 1], arow[0:1, j * 128:(j + 1) * 128], one1[0:1, 0:1], True, True,
             [arow.r[0], one1.r[0]], pb.r)
    S.tt('dve', g.ada[l][:], pb[:, 0:72], bT[:], ALU.add, [pb.r[0], bT.r[0]], g.ada[l].r)
    S.release(m)


def set_mod(g, l, sub):
    S = g.S
    ada = g.ada[l]
    sc = ada[:, (3 * sub + 1) * 8:(3 * sub + 1) * 8 + 8]
    gt = ada[:, (3 * sub + 2) * 8:(3 * sub + 2) * 8 + 8]
    A, G = g.modAs[l][sub], g.modGs[l][sub]
    S.stt(A[:], sc, 1.0, g.norms[l][:, sub, :], ALU.add, ALU.mult, [ada.r[0], g.norms[l].r[0]], A.r)
    S.ts('dve', G[:], gt, 1.0 if sub == 1 else 0.5, None, ALU.mult, None, ada.r, G.r)


def mod_shift(g, l, sub):
    return g.ada[l][:, (3 * sub) * 8:(3 * sub) * 8 + 8]


class NormTmp:
    def __init__(self, g, nm):
        S = g.S
        self.sq = S.tile([128, 8, 512], BF16, nm + 'sq')
        self.rs = S.tile([128, 512], F32, nm + 'rs')
        self.tmp = [S.tile([128, 512], F32, nm + 'tmp%d' % i) for i in range(2)]


def norm_chunk(g, l, sub, x, h, nt):
    S = g.S
    A = g.modAs[l][sub]
    sh = mod_shift(g, l, sub)
    ada = g.ada[l]
    s, r = nt.sq, nt.rs
    S.act(s[:], x[:], AF.Square, x.r, s.r)
    pb = psum(g)
    for dc in range(8):
        S.mm(pb[:], g.onesb[:], s[:, dc, :], dc == 0, dc == 7, [g.onesb.r[0], s.r[0]], pb.r)
    S.act(r[:], pb[:], AF.Sqrt, pb.r, r.r, scale=1.0 / D, bias=1e-6)
    S.op('dve', lambda e, o=r[:]: e.reciprocal(out=o, in_=o), r.r, r.r)
    for dc in range(8):
        t_ = nt.tmp[dc % 2]
        S.stt(t_[:], x[:, dc, :], A[:, dc:dc + 1], r[:], ALU.mult, ALU.mult, [x.r[0], A.r[0], r.r[0]], t_.r)
        S.act(h[:, dc, :], t_[:], AF.Identity, [t_.r[0], ada.r[0]], h.r, bias=sh[:, dc:dc + 1], scale=1.0)


def norm_pass(g, l, sub):
    S = g.S
    if (l, sub) in g.norm_done:
        return
    g.norm_done.add((l, sub))
    m = S.mark()
    xs = [S.tile([128, 8, 512], F32, 'nxs%d' % i) for i in range(2)]
    hb = [S.tile([128, 8, 512], BF16, 'nhb%d' % i) for i in range(2)]
    nt = NormTmp(g, 'np')

    def nload(tc):
        S.dma('sp', xs[tc % 2][:], g.XT[:, :, tc * 512:(tc + 1) * 512].rearrange("c p t -> p c t"),
              reads=[g.XTr[tc]], writes=xs[tc % 2].r, partial=False)
    nload(0)
    for tc in range(NCH):
        x, h = xs[tc % 2], hb[tc % 2]
        if tc + 1 < NCH:
            nload(tc + 1)
        norm_chunk(g, l, sub, x, h, nt)
        S.dma('sp', g.HT[:, :, tc * 512:(tc + 1) * 512].rearrange("c p t -> p c t"), h[:],
              reads=h.r, writes=[g.HTr[tc]], partial=False)
    S.release(m)


def ffn(g, l, sub, next_norm=None):
    S, din = g.S, g.din
    nm = 'ffn1' if sub == 0 else 'ffn2'
    norm_pass(g, l, sub)
    modG = g.modGs[l][sub]
    if g.stop == 'norm':
        return
    m = S.mark()
    FH = DFF // 2
    wgs = [S.tile([128, 8, FH], BF16, 'wg0')] * 2
    wus = [S.tile([128, 8, FH], BF16, 'wu0')] * 2
    wds = [S.tile([128, 11, D], BF16, 'wd0')] * 2
    if next_norm is not None:
        nt = NormTmp(g, 'fn')
        hN = [S.tile([128, 8, 512], BF16, 'fhN%d' % i) for i in range(2)]
        g.norm_done.add(next_norm)
    hbs = [S.tile([128, 8, 512], BF16, 'fhb%d' % i) for i in range(2)]
    at = [S.tile([128, 11, 512], BF16, 'fat%d' % i) for i in range(2)]
    sg = [S.tile([128, 512], BF16, 'fsg%d' % i) for i in range(2)]
    xs = [S.tile([128, 8, 512], F32, 'fxs%d' % i) for i in range(2)]
    it = 0

    def fload(i_, tc_):
        S.dma('sp', hbs[i_ % 2][:], g.HT[:, :, tc_ * 512:(tc_ + 1) * 512].rearrange("c p t -> p c t"),
              reads=[g.HTr[tc_]], writes=hbs[i_ % 2].r, partial=False)
        S.dma('sp', xs[i_ % 2][:], g.XT[:, :, tc_ * 512:(tc_ + 1) * 512].rearrange("c p t -> p c t"),
              reads=[g.XTr[tc_]], writes=xs[i_ % 2].r, partial=False)
    fload(0, 0)
    for half in range(2):
        f0 = half * FH
        wg, wu, wd = wgs[half], wus[half], wds[half]
        for dc in range(8):
            S.dma('pool', wg[:, dc, :], din[nm + '_w_gate'][l][dc * 128:(dc + 1) * 128, f0:f0 + FH],
                  writes=wg.r, partial=(dc > 0))
            S.dma('pool', wu[:, dc, :], din[nm + '_w_up'][l][dc * 128:(dc + 1) * 128, f0:f0 + FH],
                  writes=wu.r, partial=(dc > 0))
        for fc in range(11):
            S.dma('pool', wd[:, fc, :], din[nm + '_w_down'][l][f0 + fc * 128:f0 + (fc + 1) * 128, :],
                  writes=wd.r, partial=(fc > 0))
        for tc in range(NCH):
            hb, a, x = hbs[it % 2], at[it % 2], xs[it % 2]
            it += 1
            if it < 2 * NCH:
                fload(it, it % NCH)
            for fc in range(11):
                pg, pu = psum(g), psum(g)
                for dc in range(8):
                    S.mm(pg[:], wg[:, dc, fc * 128:(fc + 1) * 128], hb[:, dc, :], dc == 0, dc == 7,
                         [wg.r[0], hb.r[0]], pg.r)
                for dc in range(8):
                    S.mm(pu[:], wu[:, dc, fc * 128:(fc + 1) * 128], hb[:, dc, :], dc == 0, dc == 7,
                         [wu.r[0], hb.r[0]], pu.r)
                s_ = sg[fc % 2]
                S.act(s_[:], pg[:], AF.Silu, pg.r, s_.r)
                S.tt('dve', a[:, fc, :], s_[:], pu[:], ALU.mult, [s_.r[0], pu.r[0]], a.r, )
            for dc in range(8):
                py = psum(g)
                for fc in range(11):
                    S.mm(py[:], wd[:, fc, dc * 128:(dc + 1) * 128], a[:, fc, :], fc == 0, fc == 10,
                         [wd.r[0], a.r[0]], py.r)
                S.stt(x[:, dc, :], py[:], modG[:, dc:dc + 1], x[:, dc, :], ALU.mult, ALU.add,
                      [py.r[0], modG.r[0], x.r[0]], x.r)
            S.dma('sp', g.XT[:, :, tc * 512:(tc + 1) * 512].rearrange("c p t -> p c t"), x[:],
                  reads=x.r, writes=[g.XTr[tc]], partial=False)
            if half == 1 and next_norm is not None:
                h_ = hN[tc % 2]
                norm_chunk(g, next_norm[0], next_norm[1], x, h_, nt)
                S.dma('sp', g.HT[:, :, tc * 512:(tc + 1) * 512].rearrange("c p t -> p c t"), h_[:],
                      reads=h_.r, writes=[g.HTr[tc]], partial=False)
    S.release(m)


def wseg(name):
    return WSEG[name][0]


def load_win(g, l, mix, name='win'):
    S, din = g.S, g.din
    c0, c1 = WSEG[mix + '0'][0], WSEG[mix + '1'][0]
    n = c1 - c0
    w = S.tile([128, 8, n], BF16, name)
    for dc in range(8):
        S.dma('pool', w[:, dc, :], din['w_in_g'][l][dc * 128:(dc + 1) * 128, c0:c1], writes=w.r, partial=(dc > 0))
    return w, c0


def proj(g, pb, w, col, M, hb, rows0=0):
    S = g.S
    for dc in range(8):
        S.mm(pb[0:M, :], w[:, dc, col:col + M], hb[:, dc, :], dc == 0, dc == 7, [w.r[0], hb.r[0]], pb.r)


def load_chunk_inputs(g, tc, hb, tabs):
    S, din = g.S, g.din
    S.dma('sp', hb[:], g.HT[:, :, tc * 512:(tc + 1) * 512].rearrange("c p t -> p c t"),
          reads=[g.HTr[tc]], writes=hb.r, partial=False)
    for t_, nm in tabs:
        S.dma('sp', t_[:], din[nm][:, tc * 512:(tc + 1) * 512], writes=t_.r, partial=False)


def rope_to(g, dst_ap, dstR, pa, pbs, Ct, St, r0, r1, tmp1, tmp2, extra=None):
    S = g.S
    S.tt('dve', tmp1[r0:r1, :], pa[r0:r1, :], Ct[r0:r1, :], ALU.mult, [pa.r[0], Ct.r[0]], tmp1.r)
    S.tt('dve', tmp2[r0:r1, :], pbs[r0:r1, :], St[r0:r1, :], ALU.mult, [pbs.r[0], St.r[0]], tmp2.r)
    if extra is not None:
        S.tt('pool', extra[0], tmp1[r0:r1, :], tmp2[r0:r1, :], ALU.add, [tmp1.r[0], tmp2.r[0]], extra[1])
        S.cp('act', dst_ap, extra[0], extra[1], dstR, partial=True)
    else:
        S.tt('pool', dst_ap, tmp1[r0:r1, :], tmp2[r0:r1, :], ALU.add, [tmp1.r[0], tmp2.r[0]], dstR)


class AttnWork:
    def __init__(self, g, nm):
        S = g.S
        self.pt = [S.tile([128, 512], BF16, nm + 'pt%d' % i) for i in range(5)]
        self.osb = [S.tile([128, 512], F32, nm + 'osb%d' % i) for i in range(2)]
        self.o = [S.tile([64, 512], F32, nm + 'o%d' % i) for i in range(2)]
        self.ip = 0
        self.io = 0


def attn_chunk(g, wk, c, klist, qfn, kfn, vfn, scale, Kc, skew=3, mask_engs=('pool',)):
    S = g.S
    po = psum_o(g)
    n = len(klist)
    pts = {}

    def stage1(i):
        kt, lo, mask, mR, mlo, mhi = klist[i]
        ps = psum(g)
        qa, qR = qfn(lo)
        ka, kR = kfn(kt)
        S.mm(ps[:, lo:512], ka, qa, True, True, qR + kR, ps.r)
        pt = wk.pt[wk.ip % 5]
        wk.ip += 1
        S.act(pt[:, lo:512], ps[:, lo:512], AF.Exp, ps.r, pt.r, scale=scale)
        if mask is not None:
            S.tt(mask_engs[i % len(mask_engs)], pt[:, mlo:mhi], pt[:, mlo:mhi], mask, ALU.mult, pt.r + mR, pt.r)
        pts[i] = pt

    def stage2(i):
        kt, lo, mask, mR, mlo, mhi = klist[i]
        va, vR = vfn(kt)
        pt = pts.pop(i)
        S.mm(po[0:65, lo:512], va, pt[:, lo:512], i == 0, i == n - 1, vR + pt.r, po.r)

    for i in range(min(skew, n)):
        stage1(i)
    for i in range(n):
        if i + skew < n:
            stage1(i + skew)
        stage2(i)
    return po


def normalize_o(g, wk, po):
    S = g.S
    osb = wk.osb[wk.io % 2]
    o = wk.o[wk.io % 2]
    wk.io += 1
    S.cp('act', osb[0:65, :], po[0:65, :], po.r, osb.r)
    S.ts('dve', osb[64:65, :], osb[64:65, :], 1e-30, None, ALU.max, None, osb.r, osb.r)
    S.op('dve', lambda e, a=osb[64:65, :]: e.reciprocal(out=a, in_=a), osb.r, osb.r)
    pb = psum(g)
    S.mm(pb[0:64, :], g.sel64[0:65, 0:64], osb[0:65, :], True, True, [g.sel64.r[0], osb.r[0]], pb.r)
    S.tt('dve', o[0:64, :], osb[0:64, :], pb[0:64, :], ALU.mult, [osb.r[0], pb.r[0]], o.r)
    return o


def store_o(g, o, grp, h, c):
    S = g.S
    ch = 2 * grp + h // 2
    p0 = (h % 2) * 64
    S.dma('sp', g.OT[ch, p0:p0 + 64, c * 512:(c + 1) * 512], o[0:64, :], reads=o.r, writes=[g.OTr[grp][c]])


def causal_klist(g, c, masks):
    kl = []
    for kt in range(4 * c + 4):
        i = kt - 4 * c
        if i < 0:
            kl.append((kt, 0, None, [], 0, 0))
        else:
            kl.append((kt, i * 128, masks[:, i, i * 128:(i + 1) * 128], [masks.r[0]], i * 128, (i + 1) * 128))
    return kl


def moba(g, l):
    S, din = g.S, g.din
    m = S.mark()
    w, c0 = load_win(g, l, 'MOBA')
    col = lambda nm: WSEG[nm][0] - c0
    masks = S.tile([128, 8, 512], BF16, 'masks')
    S.dma('sp', masks[:], din['masks'], writes=masks.r)
    QT = [S.tile([128, T], BF16, 'mQT%d' % h) for h in range(4)]
    KT = [S.tile([128, T], BF16, 'mKT%d' % h) for h in range(4)]
    VA = S.tile([128, 32, 4, 65], BF16, 'mVA')
    ksum = S.tile([64, 4, 16], F32, 'mksum')
    kmT = S.tile([64, 4, 16], BF16, 'mkmT')
    S.memset('pool', VA[:, :, :, 64:65], 1.0, VA.r)
    for h in range(4):
        S.dma('sp', KT[h][64:80, :], din['eind'][64:80, 0, :], writes=KT[h].r)
    m2 = S.mark()
    hbs = [S.tile([128, 8, 512], BF16, 'mhb%d' % i) for i in range(2)]
    Cs = [S.tile([128, 512], F32, 'mC%d' % i) for i in range(2)]
    Ss = [S.tile([128, 512], F32, 'mS%d' % i) for i in range(2)]
    t1 = [S.tile([64, 512], F32, 'mt1%d' % i) for i in range(2)]
    t2 = [S.tile([64, 512], F32, 'mt2%d' % i) for i in range(2)]
    kf = [S.tile([64, 512], F32, 'mkf%d' % i) for i in range(2)]
    it = 0
    for tc in range(NCH):
        hb, Ct, St = hbs[tc % 2], Cs[tc % 2], Ss[tc % 2]
        load_chunk_inputs(g, tc, hb, [(Ct, 'ropeC'), (St, 'ropeS')])
        sl = slice(tc * 512, (tc + 1) * 512)
        for h in range(4):
            pa, pb_ = psum(g), psum(g)
            proj(g, pa, w, col('mq') + h * 64, 64, hb)
            proj(g, pb_, w, col('mq_s') + h * 64, 64, hb)
            rope_to(g, QT[h][0:64, sl], QT[h].r, pa, pb_, Ct, St, 0, 64, t1[it % 2], t2[it % 2])
            it += 1
            pa, pb_ = psum(g), psum(g)
            proj(g, pa, w, col('mk') + h * 64, 64, hb)
            proj(g, pb_, w, col('mk_s') + h * 64, 64, hb)
            kf_ = kf[it % 2]
            rope_to(g, KT[h][0:64, sl], KT[h].r, pa, pb_, Ct, St, 0, 64, t1[it % 2], t2[it % 2],
                    extra=(kf_[0:64, :], kf_.r))
            S.op('dve', lambda e, o=ksum[:, h, 2 * tc:2 * tc + 2], i=kf_[0:64, :].rearrange("p (a b) -> p a b", a=2):
                 e.tensor_reduce(out=o, in_=i, axis=AX.X, op=ALU.add), kf_.r, ksum.r, partial=True)
            it += 1
        for j in range(4):
            pv = psum(g)
            for dc in range(8):
                S.mm(pv[:, 0:256], hb[:, dc, j * 128:(j + 1) * 128], w[:, dc, col('mv'):col('mv') + 256],
                     dc == 0, dc == 7, [hb.r[0], w.r[0]], pv.r)
            S.cp('act', VA[:, tc * 4 + j, :, 0:64], pv[:, 0:256].rearrange("p (a b) -> p a b", a=4), pv.r, VA.r,
                 partial=True)
    S.release(m2)
    S.act(kmT[:], ksum[:], AF.Copy, ksum.r, kmT.r, scale=1.0 / 256)
    G = [S.tile([128, 16], F32, 'mG%d' % i) for i in range(2)]
    m8 = [S.tile([128, 8], F32, 'mm8%d' % i) for i in range(2)]
    NBT = [S.tile([128, 128], BF16, 'mNBT%d' % i) for i in range(4)]
    for t_ in NBT:
        S.memset('pool', t_[:], 0.0, t_.r)
    it = 0
    for h in range(4):
        for c in range(NCH):
            pt_ = psum(g)
            for j in range(4):
                qt = 4 * c + j
                own = qt // 2
                nb = NBT[it % 4]
                if own >= 4:
                    G_, m8_ = G[it % 2], m8[it % 2]
                    pg = psum(g)
                    S.mm(pg[:, 0:16], QT[h][0:64, qt * 128:(qt + 1) * 128], kmT[0:64, h, :], True, True,
                         [QT[h].r[0], kmT.r[0]], pg.r)
                    S.memset('pool', G_[:], -1e30, G_.r)
                    S.cp('dve', G_[:, 0:own], pg[:, 0:own], pg.r, G_.r)
                    S.op('dve', lambda e, o=m8_[:], i=G_[:]: e.max(out=o, in_=i), G_.r, m8_.r)
                    S.ts('dve', nb[:, 64:80], G_[:], m8_[:, 2:3], NEGB, ALU.is_lt, ALU.mult,
                         [G_.r[0], m8_.r[0]], nb.r)
                    S.memset('pool', nb[:, 64 + own:65 + own], 0.0, nb.r)
                else:
                    S.memset('pool', nb[:, 64:80], NEGB, nb.r)
                    S.memset('pool', nb[:, 64:65 + own], 0.0, nb.r)
                it += 1
                S.mm(pt_[0:80, j * 128:(j + 1) * 128], nb[:, 0:80], g.identb[:], True, True,
                     [nb.r[0], g.identb.r[0]], pt_.r)
            S.cp('act', QT[h][64:80, c * 512:(c + 1) * 512], pt_[64:80, :], pt_.r, QT[h].r, partial=True)
    wk = AttnWork(g, 'm')
    for h in range(4):
        for c in range(NCH):
            kl = causal_klist(g, c, masks)
            po = attn_chunk(g, wk, c, kl,
                            lambda lo, h=h, c=c: (QT[h][0:80, c * 512 + lo:(c + 1) * 512], [QT[h].r[0]]),
                            lambda kt, h=h: (KT[h][0:80, kt * 128:(kt + 1) * 128], [KT[h].r[0]]),
                            lambda kt, h=h: (VA[:, kt, h, :], [VA.r[0]]),
                            0.125, 80)
            o = normalize_o(g, wk, po)
            store_o(g, o, 0, h, c)
    S.release(m)


def rms_feat(g, srcs, nrm, gains, outs, sqs, rtile, width):
    S = g.S
    n = len(srcs)
    for i in range(n):
        S.act(sqs[i][:], srcs[i][:], AF.Square, srcs[i].r, sqs[i].r)
    pss = psum(g)
    for i in range(n):
        S.mm(pss[:], g.onesb[:], sqs[i][:], i == 0, i == n - 1, [g.onesb.r[0], sqs[i].r[0]], pss.r)
    S.act(rtile[:], pss[:], AF.Sqrt, pss.r, rtile.r, scale=1.0 / width, bias=1e-6)
    S.op('dve', lambda e, o=rtile[:]: e.reciprocal(out=o, in_=o), rtile.r, rtile.r)
    for i in range(n):
        S.stt(outs[i][:], srcs[i][:], gains[:, i:i + 1], rtile[:], ALU.mult, ALU.mult,
              [srcs[i].r[0], gains.r[0], rtile.r[0]], outs[i].r)


def mla(g, l):
    S, din = g.S, g.din
    m = S.mark()
    w, c0 = load_win(g, l, 'MLA')
    col = lambda nm: WSEG[nm][0] - c0
    masks = S.tile([128, 8, 512], BF16, 'masks')
    S.dma('sp', masks[:], din['masks'], writes=masks.r)
    wuq = S.tile([128, 2, 384], BF16, 'wuq')
    wuqs = S.tile([128, 2, 384], BF16, 'wuqs')
    wuk = S.tile([128, 256], BF16, 'wuk')
    wuv = S.tile([128, 256], BF16, 'wuv')
    qn = S.tile([128, 2], F32, 'qn')
    kvn = S.tile([128, 1], F32, 'kvn')
    S.dma('pool', wuq[:], din['mla_w_uq'][l].rearrange("(c p) n -> p c n", p=128), writes=wuq.r)
    S.dma('pool', wuqs[:], din['mla_w_uq_s'][l].rearrange("(c p) n -> p c n", p=128), writes=wuqs.r)
    S.dma('pool', wuk[:], din['mla_w_uk'][l], writes=wuk.r)
    S.dma('pool', wuv[:], din['mla_w_uv'][l], writes=wuv.r)
    S.dma('sp', qn[:], din['mla_qnT'][l], writes=qn.r)
    S.dma('sp', kvn[:], din['mla_kvnT'][l], writes=kvn.r)
    QT = [S.tile([128, T], BF16, 'aQT%d' % h) for h in range(4)]
    KT = [S.tile([128, T], BF16, 'aKT%d' % h) for h in range(4)]
    VA = S.tile([128, 32, 4, 65], BF16, 'aVA')
    S.memset('pool', VA[:, :, :, 64:65], 1.0, VA.r)
    m2 = S.mark()
    hbs = [S.tile([128, 8, 512], BF16, 'ahb%d' % i) for i in range(2)]
    Cs = [S.tile([128, 512], F32, 'aC%d' % i) for i in range(2)]
    Ss = [S.tile([128, 512], F32, 'aS%d' % i) for i in range(2)]
    sq = [S.tile([128, 512], BF16, 'asq%d' % i) for i in range(3)]
    cqn = [S.tile([128, 512], BF16, 'acqn%d' % i) for i in range(2)]
    ckvn = S.tile([128, 512], BF16, 'ackvn')
    rq = S.tile([128, 512], F32, 'arq')
    rkv = S.tile([128, 512], F32, 'arkv')
    t1 = [S.tile([128, 512], F32, 'at1%d' % i) for i in range(2)]
    t2 = [S.tile([128, 512], F32, 'at2%d' % i) for i in range(2)]
    kpe = S.tile([128, 512], BF16, 'akpe')
    it = 0
    for tc in range(NCH):
        hb, Ct, St = hbs[tc % 2], Cs[tc % 2], Ss[tc % 2]
        load_chunk_inputs(g, tc, hb, [(Ct, 'ropeCm'), (St, 'ropeSm')])
        sl = slice(tc * 512, (tc + 1) * 512)
        pc = [psum(g), psum(g)]
        for i in range(2):
            proj(g, pc[i], w, col('cq') + i * 128, 128, hb)
        rms_feat(g, pc, None, qn, cqn, sq[0:2], rq, 256.0)
        pk = psum(g)
        proj(g, pk, w, col('ckv'), 128, hb)
        rms_feat(g, [pk], None, kvn, [ckvn], sq[2:3], rkv, 128.0)
        for h in range(4):
            pa, pb_ = psum(g), psum(g)
            for i in range(2):
                S.mm(pa[0:96, :], wuq[:, i, h * 96:(h + 1) * 96], cqn[i][:], i == 0, i == 1,
                     [wuq.r[0], cqn[i].r[0]], pa.r)
            for i in range(2):
                S.mm(pb_[0:96, :], wuqs[:, i, h * 96:(h + 1) * 96], cqn[i][:], i == 0, i == 1,
                     [wuqs.r[0], cqn[i].r[0]], pb_.r)
            S.cp('dve', QT[h][0:64, sl], pa[0:64, :], pa.r, QT[h].r, partial=True)
            rope_to(g, QT[h][64:96, sl], QT[h].r, pa, pb_, Ct, St, 64, 96, t1[it % 2], t2[it % 2])
            it += 1
            pkn = psum(g)
            S.mm(pkn[0:64, :], wuk[:, h * 64:(h + 1) * 64], ckvn[:], True, True, [wuk.r[0], ckvn.r[0]], pkn.r)
            S.cp('act', KT[h][0:64, sl], pkn[0:64, :], pkn.r, KT[h].r, partial=True)
        pa, pb_ = psum(g), psum(g)
        proj(g, pa, w, col('kr96'), 96, hb)
        proj(g, pb_, w, col('kr96_s'), 96, hb)
        rope_to(g, kpe[64:96, :], kpe.r, pa, pb_, Ct, St, 64, 96, t1[it % 2], t2[it % 2])
        it += 1
        for h in range(4):
            S.cp('pool', KT[h][64:96, sl], kpe[64:96, :], kpe.r, KT[h].r, partial=True)
        for j in range(4):
            pv = psum(g)
            S.mm(pv[:, 0:256], ckvn[:, j * 128:(j + 1) * 128], wuv[:], True, True, [ckvn.r[0], wuv.r[0]], pv.r)
            S.cp('act', VA[:, tc * 4 + j, :, 0:64], pv[:, 0:256].rearrange("p (a b) -> p a b", a=4), pv.r, VA.r,
                 partial=True)
    S.release(m2)
    wk = AttnWork(g, 'a')
    sc = float(96 ** -0.5)
    for h in range(4):
        for c in range(NCH):
            kl = causal_klist(g, c, masks)
            po = attn_chunk(g, wk, c, kl,
                            lambda lo, h=h, c=c: (QT[h][0:96, c * 512 + lo:(c + 1) * 512], [QT[h].r[0]]),
                            lambda kt, h=h: (KT[h][0:96, kt * 128:(kt + 1) * 128], [KT[h].r[0]]),
                            lambda kt, h=h: (VA[:, kt, h, :], [VA.r[0]]),
                            sc, 96)
            o = normalize_o(g, wk, po)
            store_o(g, o, 1, h, c)
    S.release(m)


NIT = 14


def dsa(g, l):
    S, din = g.S, g.din
    m = S.mark()
    dbias = S.tile([128, 128], F32, 'dbias')
    S.dma('sp', dbias[:], din['dbias'], writes=dbias.r)
    QT = [S.tile([64, T], BF16, 'dQT%d' % h) for h in range(4)]
    KT = S.tile([64, T], BF16, 'dKT')
    VA = S.tile([128, 32, 65], BF16, 'dVA')
    IQ = S.tile([128, 3, T], BF16, 'dIQ')
    IK = S.tile([128, T], BF16, 'dIK')
    IW = S.tile([128, 32, 8], F32, 'dIW')
    S.memset('pool', VA[:, :, 64:65], 1.0, VA.r)
    m2 = S.mark()
    w, c0 = load_win(g, l, 'DSA')
    col = lambda nm: WSEG[nm][0] - c0
    hbs = [S.tile([128, 8, 512], BF16, 'dhb%d' % i) for i in range(2)]
    Cs = [S.tile([128, 512], F32, 'dC%d' % i) for i in range(2)]
    Ss = [S.tile([128, 512], F32, 'dS%d' % i) for i in range(2)]
    t1 = [S.tile([64, 512], F32, 'dt1%d' % i) for i in range(2)]
    t2 = [S.tile([64, 512], F32, 'dt2%d' % i) for i in range(2)]
    it = 0
    for tc in range(NCH):
        hb, Ct, St = hbs[tc % 2], Cs[tc % 2], Ss[tc % 2]
        load_chunk_inputs(g, tc, hb, [(Ct, 'ropeC'), (St, 'ropeS')])
        sl = slice(tc * 512, (tc + 1) * 512)
        for h in range(5):
            pa, pb_ = psum(g), psum(g)
            if h < 4:
                proj(g, pa, w, col('dq') + h * 64, 64, hb)
                proj(g, pb_, w, col('dq_s') + h * 64, 64, hb)
                dst = QT[h]
            else:
                proj(g, pa, w, col('dk'), 64, hb)
                proj(g, pb_, w, col('dk_s'), 64, hb)
                dst = KT
            rope_to(g, dst[0:64, sl], dst.r, pa, pb_, Ct, St, 0, 64, t1[it % 2], t2[it % 2])
            it += 1
        for i in range(3):
            pa = psum(g)
            proj(g, pa, w, col('diq') + i * 128, 128, hb)
            S.cp('act', IQ[:, i, sl], pa[:], pa.r, IQ.r, partial=True)
        pa = psum(g)
        proj(g, pa, w, col('dik4'), 128, hb)
        S.cp('act', IK[:, sl], pa[:], pa.r, IK.r, partial=True)
        for j in range(4):
            pv = psum(g)
            for dc in range(8):
                S.mm(pv[:, 0:64], hb[:, dc, j * 128:(j + 1) * 128], w[:, dc, col('dv'):col('dv') + 64],
                     dc == 0, dc == 7, [hb.r[0], w.r[0]], pv.r)
            S.cp('act', VA[:, tc * 4 + j, 0:64], pv[:, 0:64], pv.r, VA.r, partial=True)
            pw = psum(g)
            for dc in range(8):
                S.mm(pw[:, 0:8], hb[:, dc, j * 128:(j + 1) * 128], w[:, dc, col('diw'):col('diw') + 8],
                     dc == 0, dc == 7, [hb.r[0], w.r[0]], pw.r)
            S.cp('dve', IW[:, tc * 4 + j, :], pw[:, 0:8], pw.r, IW.r, partial=True)
    S.release(m2)
    if 'dsa_stop_proj' in g.dbg:
        S.release(m)
        return
    acc = [S.tile([128, T], F32, 'dacc%d' % i) for i in range(2)]
    rl = [S.tile([128, 512], BF16, 'drl%d' % i) for i in range(4)]
    Dg = [S.tile([128, 8, 128], BF16, 'dDg%d' % i) for i in range(2)]
    Mq = [S.tile([128, T], BF16, 'dM%d' % i) for i in range(4)]
    MT = S.tile([128, 32, 512], BF16, 'dMT')
    X = [S.tile([128, 1], F32, 'dX%d' % i) for i in range(2)]
    STEP = [S.tile([128, NIT], F32, 'dSTEP%d' % i) for i in range(2)]
    CK = S.tile([128, NIT], F32, 'dCK')
    thr = [S.tile([128, 1], F32, 'dthr%d' % i) for i in range(2)]
    cand = [S.tile([128, 1], F32, 'dcand%d' % i) for i in range(2)]
    cnt = [S.tile([128, 1], F32, 'dcnt%d' % i) for i in range(2)]
    gs = [S.tile([128, 1], F32, 'dgs%d' % i) for i in range(2)]
    for k in range(NIT):
        S.memset('pool', CK[:, k:k + 1], float(2.0 ** (-k)), CK.r)
    wk = AttnWork(g, 'd')
    irl = 0
    for c in range(NCH if 'dsa_c1' not in g.dbg else int(g.dsa_nc)):
        for jp in range(2):
            js = (2 * jp, 2 * jp + 1)
            for j in js:
                qt = 4 * c + j
                nk = (qt + 1) * 128
                a_, D_ = acc[j % 2], Dg[j % 2]
                for hh in range(8):
                    S.ts('pool', D_[:, hh, :], g.identb[:], IW[:, qt, hh:hh + 1], None, ALU.mult, None,
                         [g.identb.r[0], IW.r[0]], D_.r)
                for kc in range((nk + 511) // 512):
                    cols = min(512, nk - kc * 512)
                    ks = slice(kc * 512, kc * 512 + cols)
                    pacc = psum_o(g)
                    rls = {}

                    def st1(hh):
                        pb = 32 * (hh % 3)
                        ps = psum(g)
                        S.mm(ps[:, 0:cols], IQ[pb:pb + 32, hh // 3, qt * 128:(qt + 1) * 128], IK[pb:pb + 32, ks],
                             True, True, [IQ.r[0], IK.r[0]], ps.r)
                        r_ = rl[st1.i % 4]
                        st1.i += 1
                        if hh % 2 == 0:
                            S.act(r_[:, 0:cols], ps[:, 0:cols], AF.Relu, ps.r, r_.r)
                        else:
                            S.ts('dve', r_[:, 0:cols], ps[:, 0:cols], 0.0, None, ALU.max, None, ps.r, r_.r)
                        rls[hh] = r_
                    st1.i = irl

                    def st2(hh):
                        r_ = rls.pop(hh)
                        S.mm(pacc[:, 0:cols], D_[:, hh, :], r_[:, 0:cols], hh == 0, hh == 7, [D_.r[0], r_.r[0]],
                             pacc.r)
                    st1(0)
                    st1(1)
                    for hh in range(8):
                        if hh + 2 < 8:
                            st1(hh + 2)
                        st2(hh)
                    irl = st1.i
                    S.cp('dve', a_[:, ks], pacc[:, 0:cols], pacc.r, a_.r, partial=True)
                S.tt('dve', a_[:, qt * 128:nk], a_[:, qt * 128:nk], dbias[:], ALU.add, [a_.r[0], dbias.r[0]], a_.r)
            act_js = []
            for j in js:
                qt = 4 * c + j
                a_, X_, ST_, cd_ = acc[j % 2], X[j % 2], STEP[j % 2], cand[j % 2]
                if qt >= 2 and 'dsa_nobisect' not in g.dbg:
                    S.op('dve', lambda e, o=X_[:], i=a_[:, 0:qt * 128]: e.tensor_reduce(
                        out=o, in_=i, axis=AX.X, op=ALU.max, apply_absolute_value=True), a_.r, X_.r)
                    S.ts('dve', X_[:], X_[:], 1.001, 1e-6, ALU.mult, ALU.add, X_.r, X_.r)
                    chainB = len(act_js) == 1
                    S.ts('dve', ST_[:], CK[:], X_[:, 0:1], -1.0 if chainB else 1.0, ALU.mult, ALU.mult,
                         [CK.r[0], X_.r[0]], ST_.r)
                    S.memset('pool', cd_[:], 0.0, cd_.r)
                    act_js.append(j)
            for k in range(NIT):
                for ci, j in enumerate(act_js):
                    qt = 4 * c + j
                    nk = (qt + 1) * 128
                    a_, ST_, cd_, cn_, gs_, Mj = (acc[j % 2], STEP[j % 2], cand[j % 2], cnt[j % 2], gs[j % 2], Mq[j])
                    if ci == 0:
                        S.ts('dve', Mj[:, 0:nk], a_[:, 0:nk], cd_[:, 0:1], None, ALU.is_ge, ALU.add,
                             [a_.r[0], cd_.r[0]], [Mj.r[0], cn_.r[0]], accum_out=cn_[:])
                        S.ts('dve', gs_[:], cn_[:], 255.5, 0.5, ALU.is_ge, ALU.subtract, cn_.r, gs_.r)
                        S.stt(cd_[:], gs_[:], ST_[:, k:k + 1], cd_[:], ALU.mult, ALU.add,
                              [gs_.r[0], ST_.r[0], cd_.r[0]], cd_.r)
                    else:
                        S.act(Mj[:, 0:nk], a_[:, 0:nk], AF.Sign, [a_.r[0], cd_.r[0]], [Mj.r[0], cn_.r[0]],
                              bias=cd_[:, 0:1], scale=1.0, accum_out=cn_[:])
                        S.ts('pool', gs_[:], cn_[:], float(511 - nk), 0.5, ALU.is_ge, ALU.subtract, cn_.r, gs_.r)
                        S.ts('pool', cd_[:], gs_[:], ST_[:, k:k + 1], cd_[:, 0:1], ALU.mult, ALU.add,
                             [gs_.r[0], ST_.r[0], cd_.r[0]], cd_.r)
            for ci, j in enumerate(act_js):
                X_, cd_, th_ = X[j % 2], cand[j % 2], thr[j % 2]
                if ci == 1:
                    S.ts('dve', cd_[:], cd_[:], -1.0, None, ALU.mult, None, cd_.r, cd_.r)
                S.stt(th_[:], X_[:], float(-(2.0 ** (-NIT))), cd_[:], ALU.mult, ALU.add, [X_.r[0], cd_.r[0]], th_.r)
            for j in js:
                qt = 4 * c + j
                nk = (qt + 1) * 128
                a_, th_, Mj = acc[j % 2], thr[j % 2], Mq[j]
                if j in act_js:
                    S.ts('dve', Mj[:, 0:nk], a_[:, 0:nk], th_[:, 0:1], None, ALU.is_ge, None, [a_.r[0], th_.r[0]],
                         Mj.r)
                else:
                    S.ts('dve', Mj[:, 0:nk], a_[:, 0:nk], -1e29, None, ALU.is_ge, None, a_.r, Mj.r)
        for kt in range(4 * c + 4):
            i0 = max(0, kt - 4 * c)
            pt_ = psum(g)
            for j in range(i0, 4):
                S.mm(pt_[:, j * 128:(j + 1) * 128], Mq[j][:, kt * 128:(kt + 1) * 128], g.identb[:], True, True,
                     [Mq[j].r[0], g.identb.r[0]], pt_.r)
            S.cp('act', MT[:, kt, i0 * 128:512], pt_[:, i0 * 128:512], pt_.r, MT.r, partial=True)
        for h in range(4 if 'dsa_noattn' not in g.dbg else 0):
            kl = []
            for kt in range(4 * c + 4):
                lo = max(0, kt - 4 * c) * 128
                kl.append((kt, lo, MT[:, kt, lo:512], [MT.r[0]], lo, 512))
            po = attn_chunk(g, wk, c, kl,
                            lambda lo, h=h, c=c: (QT[h][0:64, c * 512 + lo:(c + 1) * 512], [QT[h].r[0]]),
                            lambda kt: (KT[0:64, kt * 128:(kt + 1) * 128], [KT.r[0]]),
                            lambda kt: (VA[:, kt, :], [VA.r[0]]),
                            0.125, 64, mask_engs=('pool', 'dve'))
            o = normalize_o(g, wk, po)
            store_o(g, o, 3, h, c)
    S.release(m)


def nsa(g, l):
    S, din = g.S, g.din
    m = S.mark()
    w, c0 = load_win(g, l, 'NSA')
    col = lambda nm: WSEG[nm][0] - c0
    masks = S.tile([128, 8, 512], BF16, 'masks')
    S.dma('sp', masks[:], din['masks'], writes=masks.r)
    QT = [S.tile([128, T], BF16, 'nQT%d' % h) for h in range(4)]
    KS = S.tile([128, T], BF16, 'nKS')
    KW = S.tile([64, T], BF16, 'nKW')
    VAs = S.tile([128, 32, 65], BF16, 'nVAs')
    VAw = S.tile([128, 32, 65], BF16, 'nVAw')
    KcmpT = S.tile([64, 256], BF16, 'nKcmp')
    VAc = S.tile([128, 2, 65], BF16, 'nVAc')
    c2s = S.tile([128, 2, 65], BF16, 'nc2s')
    S.dma('sp', c2s[:], din['c2s'], writes=c2s.r)
    S.dma('sp', KS[64:128, :], din['eind'][64:128, 1, :], writes=KS.r)
    S.memset('pool', VAs[:, :, 64:65], 1.0, VAs.r)
    S.memset('pool', VAw[:, :, 64:65], 1.0, VAw.r)
    S.memset('pool', VAc[:], 0.0, VAc.r)
    S.memset('pool', VAc[:, :, 64:65], 1.0, VAc.r)
    S.memset('pool', KcmpT[:], 0.0, KcmpT.r)
    m2 = S.mark()
    KC = S.tile([64, T], F32, 'nKC')
    VC = S.tile([64, T], F32, 'nVC')
    m3 = S.mark()
    hbs = [S.tile([128, 8, 512], BF16, 'nhb%d' % i) for i in range(2)]
    Cs = [S.tile([128, 512], F32, 'nC%d' % i) for i in range(2)]
    Ss = [S.tile([128, 512], F32, 'nS%d' % i) for i in range(2)]
    t1 = [S.tile([64, 512], F32, 'nt1%d' % i) for i in range(2)]
    t2 = [S.tile([64, 512], F32, 'nt2%d' % i) for i in range(2)]
    it = 0
    for tc in range(NCH if 'nsa_noloop' not in g.dbg else 0):
        hb, Ct, St = hbs[tc % 2], Cs[tc % 2], Ss[tc % 2]
        load_chunk_inputs(g, tc, hb, [(Ct, 'ropeC'), (St, 'ropeS')])
        sl = slice(tc * 512, (tc + 1) * 512)
        for h in range(7):
            pa, pb_ = psum(g), psum(g)
            if h < 4:
                nm, off, dst = 'nq', h * 64, QT[h]
            else:
                nm, off, dst = (('nkc', 0, KC), ('nks', 0, KS), ('nkw', 0, KW))[h - 4]
            proj(g, pa, w, col(nm) + off, 64, hb)
            proj(g, pb_, w, col(nm + '_s') + off, 64, hb)
            rope_to(g, dst[0:64, sl], dst.r, pa, pb_, Ct, St, 0, 64, t1[it % 2], t2[it % 2])
            it += 1
        pa = psum(g)
        proj(g, pa, w, col('nvc'), 64, hb)
        S.cp('act', VC[0:64, sl], pa[0:64, :], pa.r, VC.r, partial=True)
        for j in range(4):
            pv = psum(g)
            for dc in range(8):
                S.mm(pv[:, 0:128], hb[:, dc, j * 128:(j + 1) * 128], w[:, dc, col('nvs'):col('nvs') + 128],
                     dc == 0, dc == 7, [hb.r[0], w.r[0]], pv.r)
            S.cp('act', VAs[:, tc * 4 + j, 0:64], pv[:, 0:64], pv.r, VAs.r, partial=True)
            S.cp('act', VAw[:, tc * 4 + j, 0:64], pv[:, 64:128], pv.r, VAw.r, partial=True)
    S.release(m3)
    if 'nsa_stop1' in g.dbg:
        S.release(m)
        return
    peT = S.tile([64, 2, 32], F32, 'npe')
    S.dma('sp', peT[:], din['nsa_peT'][l], writes=peT.r)
    w1 = S.tile([64, 32, 256], BF16, 'nw1')
    w2 = S.tile([128, 2, 64], BF16, 'nw2')
    Xall = S.tile([64, 32, 256], BF16, 'nXall')
    hid = [S.tile([128, 256], BF16, 'nhid%d' % i) for i in range(2)]
    S.memset('pool', Xall[:], 0.0, Xall.r)
    for x in range(2):
        src = KC if x == 0 else VC
        S.dma('pool', w1[:], din['nsa_w1'][l, x].rearrange("(j d) m -> d j m", d=64), writes=w1.r, partial=False)
        S.dma('pool', w2[:], din['nsa_w2'][l, x].rearrange("(c p) n -> p c n", p=128), writes=w2.r, partial=False)
        for j in range(32):
            S.ts('dve' if j % 2 == 0 else 'pool', Xall[:, j, 0:255], src[0:64, j:j + 16 * 254 + 1:16],
                 peT[:, x, j:j + 1], None, ALU.add, None, [src.r[0], peT.r[0]], Xall.r)
        for mc in range(2):
            ph = psum(g)
            for j in range(32):
                S.mm(ph[:, 0:256], w1[:, j, mc * 128:(mc + 1) * 128], Xall[:, j, :], j == 0, j == 31,
                     [w1.r[0], Xall.r[0]], ph.r)
            S.act(hid[mc][:], ph[:, 0:256], AF.Silu, ph.r, hid[mc].r)
        if x == 0:
            pk = psum(g)
            for mc in range(2):
                S.mm(pk[0:64, 0:256], w2[:, mc, :], hid[mc][:], mc == 0, mc == 1, [w2.r[0], hid[mc].r[0]], pk.r)
            S.cp('act', KcmpT[:, 0:255], pk[0:64, 0:255], pk.r, KcmpT.r)
        else:
            for nt in range(2):
                pv = psum(g)
                for mc in range(2):
                    S.mm(pv[:, 0:64], hid[mc][:, nt * 128:(nt + 1) * 128], w2[:, mc, :], mc == 0, mc == 1,
                         [hid[mc].r[0], w2.r[0]], pv.r)
                S.cp('act', VAc[:, nt, 0:64], pv[:, 0:64], pv.r, VAc.r)
    S.release(m2)
    if 'nsa_stop2' in g.dbg:
        S.release(m)
        return
    mcmp = S.tile([128, 2, T], BF16, 'nmcmp')
    S.dma('sp', mcmp[:], din['mcmp'], writes=mcmp.r)
    nA = S.tile([128, 32, 64], F32, 'nA')
    nB = S.tile([128, 32, 64], F32, 'nB')
    S.dma('sp', nA[:], din['nsaA'], writes=nA.r)
    S.dma('sp', nB[:], din['nsaB'], writes=nB.r)
    hbs = [S.tile([128, 8, 512], BF16, 'nhb2%d' % i) for i in range(1)]
    Gt = [S.tile([64, 12, 512], F32, 'nGt%d' % i) for i in range(1)]
    imp = [S.tile([128, 4, 64], F32, 'nimp%d' % i) for i in range(2)]
    rd = [S.tile([128, 1], F32, 'nrd%d' % i) for i in range(2)]
    acc = [S.tile([64, 512], F32, 'nacc%d' % i) for i in range(4)]
    m8a = [S.tile([128, 8], F32, 'nm8a%d' % i) for i in range(2)]
    m8b = [S.tile([128, 8], F32, 'nm8b%d' % i) for i in range(2)]
    imr = [S.tile([128, 64], F32, 'nimr%d' % i) for i in range(2)]
    NBT = [S.tile([128, 128], BF16, 'nNBT%d' % i) for i in range(4)]
    nbs = S.tile([128, 512], BF16, 'nnbs')
    for t_ in NBT:
        S.memset('pool', t_[:], 0.0, t_.r)
    wk = AttnWork(g, 'n')
    ird = 0
    load_chunk_inputs(g, 0, hbs[0], [])
    for c in range(NCH):
        hb, G_, imp_ = hbs[0], Gt[0], imp[c % 2]
        for gi in range(12):
            pa = psum(g)
            proj(g, pa, w, col('ngrep') + gi * 64, 64, hb)
            S.act(G_[:, gi, :], pa[0:64, :], AF.Sigmoid, pa.r, G_.r)
        if c + 1 < NCH:
            load_chunk_inputs(g, c + 1, hb, [])
        ntl = [0] if c <= 3 else [0, 1]
        for h in range(4):
            po = psum_o(g)
            pts = []
            for ii, nt in enumerate(ntl):
                ps = psum(g)
                S.mm(ps[:], KcmpT[0:64, nt * 128:(nt + 1) * 128], QT[h][0:64, c * 512:(c + 1) * 512], True, True,
                     [KcmpT.r[0], QT[h].r[0]], ps.r)
                pt = wk.pt[wk.ip % 5]
                wk.ip += 1
                S.act(pt[:], ps[:], AF.Exp, ps.r, pt.r, scale=0.125)
                S.tt('pool', pt[:], pt[:], mcmp[:, nt, c * 512:(c + 1) * 512], ALU.mult, [pt.r[0], mcmp.r[0]], pt.r)
                S.mm(po[0:65, :], VAc[:, nt, :], pt[:], ii == 0, ii == len(ntl) - 1, [VAc.r[0], pt.r[0]], po.r)
                pts.append(pt)
            for j in range(4):
                pi = psum(g)
                for ii, nt in enumerate(ntl):
                    S.mm(pi[:, 0:65], pts[ii][:, j * 128:(j + 1) * 128], c2s[:, nt, :], ii == 0, ii == len(ntl) - 1,
                         [pts[ii].r[0], c2s.r[0]], pi.r)
                r_ = rd[ird % 2]
                ird += 1
                S.ts('dve', r_[:], pi[:, 64:65], 1e-30, None, ALU.max, None, pi.r, r_.r)
                S.op('dve', lambda e, a=r_[:]: e.reciprocal(out=a, in_=a), r_.r, r_.r)
                if h == 0:
                    S.ts('dve', imp_[:, j, :], pi[:, 0:64], r_[:, 0:1], None, ALU.mult, None, [pi.r[0], r_.r[0]],
                         imp_.r)
                else:
                    S.stt(imp_[:, j, :], pi[:, 0:64], r_[:, 0:1], imp_[:, j, :], ALU.mult, ALU.add,
                          [pi.r[0], r_.r[0], imp_.r[0]], imp_.r)
            o = normalize_o(g, wk, po)
            S.tt('dve', acc[h][:], o[0:64, :], G_[:, h * 3 + 0, :], ALU.mult, [o.r[0], G_.r[0]], acc[h].r)
        pt_ = psum(g)
        for j in range(4):
            qt = 4 * c + j
            im, a8, b8 = imr[j % 2], m8a[j % 2], m8b[j % 2]
            nb = NBT[j]
            S.tt('dve', im[:], imp_[:, j, :], nA[:, qt, :], ALU.mult, [imp_.r[0], nA.r[0]], im.r)
            S.tt('dve', im[:], im[:], nB[:, qt, :], ALU.add, [im.r[0], nB.r[0]], im.r)
            S.op('dve', lambda e, o_=a8[:], i=im[:]: e.max(out=o_, in_=i), im.r, a8.r)
            S.op('dve', lambda e, o_=nb[:, 0:64], r=a8[:], v=im[:]: e.match_replace(out=o_, in_to_replace=r,
                                                                                  in_values=v, imm_value=-1e30),
                 [a8.r[0], im.r[0]], nb.r)
            S.op('dve', lambda e, o_=b8[:], i=nb[:, 0:64]: e.max(out=o_, in_=i), nb.r, b8.r)
            S.ts('dve', nb[:, 64:128], im[:], b8[:, 7:8], NEGB, ALU.is_lt, ALU.mult, [im.r[0], b8.r[0]], nb.r)
            S.memset('pool', nb[:, 0:64], 0.0, nb.r)
            S.mm(pt_[:, j * 128:(j + 1) * 128], nb[:], g.identb[:], True, True, [nb.r[0], g.identb.r[0]], pt_.r)
        S.cp('act', nbs[64:128, :], pt_[64:128, :], pt_.r, nbs.r)
        for h in range(4):
            S.cp('pool', QT[h][64:128, c * 512:(c + 1) * 512], nbs[64:128, :], nbs.r, QT[h].r, partial=True)
        for h in range(4):
            kl = causal_klist(g, c, masks)
            po = attn_chunk(g, wk, c, kl,
                            lambda lo, h=h, c=c: (QT[h][:, c * 512 + lo:(c + 1) * 512], [QT[h].r[0]]),
                            lambda kt: (KS[:, kt * 128:(kt + 1) * 128], [KS.r[0]]),
                            lambda kt: (VAs[:, kt, :], [VAs.r[0]]),
                            0.125, 128)
            o = normalize_o(g, wk, po)
            S.tt('dve', o[0:64, :], o[0:64, :], G_[:, h * 3 + 1, :], ALU.mult, [o.r[0], G_.r[0]], o.r)
            S.tt('pool', acc[h][:], acc[h][:], o[0:64, :], ALU.add, [acc[h].r[0], o.r[0]], acc[h].r)
            kl = []
            for kt in range(max(0, 4 * c - 4), 4 * c + 4):
                r = kt - 4 * c
                if r < 0:
                    kl.append((kt, 0, masks[:, 8 + r, :], [masks.r[0]], 0, 512))
                else:
                    kl.append((kt, r * 128, masks[:, r, r * 128:(r + 1) * 128], [masks.r[0]], r * 128, (r + 1) * 128))
            po = attn_chunk(g, wk, c, kl,
                            lambda lo, h=h, c=c: (QT[h][0:64, c * 512 + lo:(c + 1) * 512], [QT[h].r[0]]),
                            lambda kt: (KW[0:64, kt * 128:(kt + 1) * 128], [KW.r[0]]),
                            lambda kt: (VAw[:, kt, :], [VAw.r[0]]),
                            0.125, 64)
            o = normalize_o(g, wk, po)
            S.tt('dve', o[0:64, :], o[0:64, :], G_[:, h * 3 + 2, :], ALU.mult, [o.r[0], G_.r[0]], o.r)
            S.tt('pool', acc[h][:], acc[h][:], o[0:64, :], ALU.add, [acc[h].r[0], o.r[0]], acc[h].r)
            store_o(g, acc[h], 2, h, c)
    S.release(m)


def wout(g, l, next_norm=None):
    S, din = g.S, g.din
    m = S.mark()
    wo = S.tile([128, 8, D], BF16, 'wo')
    S.dma('pool', wo[:], din['w_out'][l].rearrange("(c p) n -> p c n", p=128), writes=wo.r)
    gn = S.tile([128, 8], F32, 'gn')
    S.dma('sp', gn[:], din['gnT'][l], writes=gn.r)
    ots = [S.tile([128, 8, 512], F32, 'wot%d' % i) for i in range(2)]
    xs = [S.tile([128, 8, 512], F32, 'wxs%d' % i) for i in range(2)]
    ys = [S.tile([128, 8, 512], BF16, 'wy%d' % i) for i in range(2)]
    sq = [S.tile([128, 2, 512], BF16, 'wsq%d' % i) for i in range(2)]
    rt = [S.tile([128, 512], F32, 'wrt%d' % i) for i in range(2)]
    modG = g.modGs[l][1]
    if next_norm is not None:
        nt = NormTmp(g, 'wn')
        hN = [S.tile([128, 8, 512], BF16, 'whN%d' % i) for i in range(2)]
        g.norm_done.add(next_norm)
    it = 0

    def wload(tc_):
        S.dma('sp', ots[tc_ % 2][:], g.OT[:, :, tc_ * 512:(tc_ + 1) * 512].rearrange("c p t -> p c t"),
              reads=[g.OTr[gi][tc_] for gi in range(4)], writes=ots[tc_ % 2].r, partial=False)
        S.dma('sp', xs[tc_ % 2][:], g.XT[:, :, tc_ * 512:(tc_ + 1) * 512].rearrange("c p t -> p c t"),
              reads=[g.XTr[tc_]], writes=xs[tc_ % 2].r, partial=False)
    wload(0)
    for tc in range(NCH):
        ot, x, y = ots[tc % 2], xs[tc % 2], ys[tc % 2]
        if tc + 1 < NCH:
            wload(tc + 1)
        for grp in range(4):
            s_, r_ = sq[it % 2], rt[it % 2]
            it += 1
            S.act(s_[:], ot[:, 2 * grp:2 * grp + 2, :], AF.Square, ot.r, s_.r)
            pss = psum(g)
            for i in range(2):
                S.mm(pss[:], g.onesb[:], s_[:, i, :], i == 0, i == 1, [g.onesb.r[0], s_.r[0]], pss.r)
            S.act(r_[:], pss[:], AF.Sqrt, pss.r, r_.r, scale=1.0 / 256, bias=1e-6)
            S.op('dve', lambda e, o=r_[:]: e.reciprocal(out=o, in_=o), r_.r, r_.r)
            for i in range(2):
                ch = 2 * grp + i
                S.stt(y[:, ch, :], ot[:, ch, :], gn[:, ch:ch + 1], r_[:], ALU.mult, ALU.mult,
                      [ot.r[0], gn.r[0], r_.r[0]], y.r)
        for dc in range(8):
            py = psum(g)
            for mc in range(8):
                S.mm(py[:], wo[:, mc, dc * 128:(dc + 1) * 128], y[:, mc, :], mc == 0, mc == 7,
                     [wo.r[0], y.r[0]], py.r)
            S.stt(x[:, dc, :], py[:], modG[:, dc:dc + 1], x[:, dc, :], ALU.mult, ALU.add,
                  [py.r[0], modG.r[0], x.r[0]], x.r)
        S.dma('sp', g.XT[:, :, tc * 512:(tc + 1) * 512].rearrange("c p t -> p c t"), x[:],
              reads=x.r, writes=[g.XTr[tc]], partial=False)
        if next_norm is not None:
            h_ = hN[tc % 2]
            norm_chunk(g, next_norm[0], next_norm[1], x, h_, nt)
            S.dma('sp', g.HT[:, :, tc * 512:(tc + 1) * 512].rearrange("c p t -> p c t"), h_[:],
                  reads=h_.r, writes=[g.HTr[tc]], partial=False)
    S.release(m)


def mixer(g, l):
    S = g.S
    norm_pass(g, l, 1)
    if 'skipmoba' not in g.dbg:
        moba(g, l)
    if g.stop == 'moba':
        return
    if 'skipmla' not in g.dbg:
        mla(g, l)
    if g.stop == 'mla':
        return
    if 'skipdsa' not in g.dbg:
        dsa(g, l)
    if g.stop == 'dsa':
        return
    if 'skipnsa' not in g.dbg:
        nsa(g, l)
    if g.stop == 'nsa':
        return
    wout(g, l, next_norm=(l, 2))


def epilogue(g):
    S, din = g.S, g.din
    m = S.mark()
    fn = S.tile([128, D], F32, 'fnb')
    S.dma('sp', fn[:], din['fnorm_b'], writes=fn.r)
    xs = [S.tile([128, 8, 512], F32, 'exs%d' % i) for i in range(2)]
    xt = [S.tile([128, D], F32, 'ext%d' % i) for i in range(2)]
    junk = S.tile([128, D], F32, 'ejunk')
    ss = [S.tile([128, 1], F32, 'ess%d' % i) for i in range(2)]
    it = 0
    for tc in range(NCH):
        x = xs[tc % 2]
        S.dma('sp', x[:], g.XT[:, :, tc * 512:(tc + 1) * 512].rearrange("c p t -> p c t"),
              reads=[g.XTr[tc]], writes=x.r, partial=False)
        for j in range(4):
            o, s_ = xt[it % 2], ss[it % 2]
            it += 1
            for hb in range(2):
                pb = psum(g)
                for dd in range(4):
                    dc = hb * 4 + dd
                    S.tr(pb[:, dd * 128:(dd + 1) * 128], x[:, dc, j * 128:(j + 1) * 128], g.ident[:],
                         [x.r[0], g.ident.r[0]], pb.r)
                S.cp('act' if hb == 0 else 'dve', o[:, hb * 512:(hb + 1) * 512], pb[:], pb.r, o.r)
            S.act(junk[:], o[:], AF.Square, o.r, [junk.r[0], s_.r[0]], accum_out=s_[:])
            S.act(s_[:], s_[:], AF.Sqrt, s_.r, s_.r, scale=1.0 / D, bias=1e-6)
            S.op('dve', lambda e, a=s_[:]: e.reciprocal(out=a, in_=a), s_.r, s_.r)
            S.stt(o[:], o[:], s_[:, 0:1], fn[:], ALU.mult, ALU.mult, [o.r[0], s_.r[0], fn.r[0]], o.r)
            tt_ = tc * 4 + j
            S.dma('sp', g.out[tt_ * 128:(tt_ + 1) * 128, :], o[:], reads=o.r, writes=[g.OUTr[tc]], is_output=True)
    S.release(m)


_CACHE = {}


def prep_inputs(inp):
    f = lambda a: np.ascontiguousarray(np.asarray(a, np.float32))
    sh = {}
    L = 2
    sh['ada_w'] = f(inp['ada_w'])
    sh['ada_bT'] = np.stack([pcol(inp['ada_b'][l], 72) for l in range(L)])
    sh['normsT'] = np.stack([np.stack([pcol(inp[k][l], 8) for k in ('ffn1_norm', 'mix_norm', 'ffn2_norm')], 1)
                             for l in range(L)])
    sh['fnorm_b'] = np.ascontiguousarray(np.broadcast_to(f(inp['final_norm'])[None, :], (128, D)))
    for nm in ('ffn1', 'ffn2'):
        for w in ('_w_gate', '_w_up', '_w_down'):
            sh[nm + w] = f(inp[nm + w])
    sh['w_in_g'] = np.ascontiguousarray(f(inp['w_in'])[:, :, WIN_IDX])
    sh['w_out'] = f(inp['w_out'])
    sh['gnT'] = np.stack([pcol(np.asarray(inp['group_norm'][l]).reshape(-1), 8) for l in range(L)])
    sh['mla_qnT'] = np.stack([pcol(inp['mla_q_norm'][l], 2) for l in range(L)])
    sh['mla_kvnT'] = np.stack([pcol(inp['mla_kv_norm'][l], 1) for l in range(L)])
    uq = f(inp['mla_w_uq'])
    sh['mla_w_uq'] = uq
    sidx = np.arange(384).reshape(4, 96).copy()
    for h in range(4):
        sidx[h, 64:96] = _swap(sidx[h, 64:96], 16)
    sh['mla_w_uq_s'] = np.ascontiguousarray(uq[:, :, sidx.reshape(-1)])
    sh['mla_w_uk'] = f(inp['mla_w_uk'])
    sh['mla_w_uv'] = f(inp['mla_w_uv'])
    pe = np.stack([f(inp['nsa_pe_k']), f(inp['nsa_pe_v'])], 1)
    sh['nsa_peT'] = np.ascontiguousarray(pe.transpose(0, 3, 1, 2))
    sh['nsa_w1'] = np.stack([f(inp['nsa_cmp_k_w1']), f(inp['nsa_cmp_v_w1'])], 1)
    sh['nsa_w2'] = np.stack([f(inp['nsa_cmp_k_w2']), f(inp['nsa_cmp_v_w2'])], 1)
    C, Sg, Cm, Sm = rope_tabs()
    sh.update(ropeC=C, ropeS=Sg, ropeCm=Cm, ropeSm=Sm)
    sh.update(const_tables())
    x = f(inp['x'])
    c = f(inp['c'])
    maps = []
    for b in range(8):
        mp = dict(sh)
        mp['x'] = x[b]
        mp['cT'] = pcol(c[b], 8)
        maps.append(mp)
    return maps


def kernel(**inputs):
    maps = prep_inputs(inputs)
    if 'nc' not in _CACHE:
        _CACHE['nc'] = build()
    res = run_bass_kernel_spmd(_CACHE['nc'], maps, core_ids=list(range(8)))
    return np.stack([np.asarray(r['out'], np.float32) for r in res.results], 0)
```

```python
import numpy as np
import concourse.bass as bass
import concourse.mybir as mybir
from concourse.bass_utils import run_bass_kernel_spmd

F32 = mybir.dt.float32
BF16 = mybir.dt.bfloat16
AF = mybir.ActivationFunctionType
ALU = mybir.AluOpType
AX = mybir.AxisListType

ENGS = ['pe', 'act', 'dve', 'pool', 'sp']
SEG = 16000
T = 4096
D = 1024
DFF = 2816
NCH = 8
NEGB = -30000.0
NPSB = 7


class Res:
    __slots__ = ('name', 'wr', 'rd', 'dsem', 'dcnt', 'psum')

    def __init__(self, name):
        self.name = name
        self.psum = False
        self.wr = {}
        self.rd = {}
        self.dsem = None
        self.dcnt = 0


class TT:
    def __init__(self, t, name, nslots=1):
        self.t = t
        self.name = name
        self.r = [Res("%s.%d" % (name, i)) for i in range(nslots)]

    def __getitem__(self, idx):
        return self.t[idx]


class Sched:
    def __init__(self, nc):
        self.nc = nc
        self.q = {e: [] for e in ENGS}
        self.cnt = {e: 0 for e in ENGS}
        self.waited = {e: {} for e in ENGS}
        self.sems = {}
        self.semctx = []
        self.out_events = {}
        self.dma_tot = {}
        self.uid = 0
        self.sb_base = 16640
        self.sb_off = self.sb_base
        self.sb_lim = 229376 - 64
        self.sb_peak = 0
        self.live = []
        self.free_dsems = []

    def tile(self, shape, dtype, name, nslots=1):
        esz = 2 if dtype == BF16 else 4
        n = 1
        for s in shape[1:]:
            n *= s
        size = (n * esz + 63) // 64 * 64
        self.uid += 1
        t = self.nc.alloc_sbuf_tensor_at("%s_%d" % (name, self.uid), list(shape), dtype, offset=self.sb_off)
        self.sb_off += size
        assert self.sb_off <= self.sb_lim, ("SBUF overflow", name, self.sb_off)
        self.sb_peak = max(self.sb_peak, self.sb_off)
        tt_ = TT(t, name, nslots)
        self.live.append((self.sb_off - size, tt_))
        return tt_

    def mark(self):
        return self.sb_off

    def release(self, m):
        self.barrier()
        self.sb_off = m
        keep = []
        for off, tt_ in self.live:
            if off >= m:
                for r in tt_.r:
                    if r.dsem is not None:
                        self.free_dsems.append((r.dsem, r.dcnt))
                        r.dsem = None
            else:
                keep.append((off, tt_))
        self.live = keep

    def sem(self, key):
        s = self.sems.get(key)
        if s is None:
            ctx = self.nc.semaphore("s%d" % len(self.sems))
            s = ctx.__enter__()
            self.semctx.append(ctx)
            self.sems[key] = s
        return s

    def _collect(self, eng, reads, writes, partial):
        deps = {}
        for r in reads:
            for k, v in r.wr.items():
                if deps.get(k, 0) < v:
                    deps[k] = v
            if r.psum:
                for k, v in r.rd.items():
                    if k[0] != eng and deps.get(k, 0) < v:
                        deps[k] = v
        for w in writes:
            if not partial:
                for k, v in w.wr.items():
                    if deps.get(k, 0) < v:
                        deps[k] = v
            for k, v in w.rd.items():
                if deps.get(k, 0) < v:
                    deps[k] = v
        wl = []
        wd = self.waited[eng]
        for k, v in deps.items():
            if eng == 'pe' and k[0] == 'pe':
                continue
            if wd.get(k, 0) < v:
                wd[k] = v
                wl.append((self.sem(k), v))
        return wl

    def op(self, eng, fn, reads=(), writes=(), partial=False):
        wl = self._collect(eng, reads, writes, partial)
        self.cnt[eng] += 1
        n = self.cnt[eng]
        key = (eng, (n - 1) // SEG)
        val = (n - 1) % SEG + 1
        self.q[eng].append((wl, fn, (self.sem(key), 1)))
        for r in reads:
            if r.rd.get(key, 0) < val:
                r.rd[key] = val
        for w in writes:
            if w.wr.get(key, 0) < val:
                w.wr[key] = val

    def dma(self, eng, out, in_, reads=(), writes=(), partial=True, is_output=False, **kw):
        wl = self._collect(eng, reads, writes, partial)
        w0 = writes[0]
        if w0.dsem is None:
            if self.free_dsems:
                w0.dsem, w0.dcnt = self.free_dsems.pop()
                if w0.dcnt > 40000:
                    self.uid += 1
                    w0.dsem, w0.dcnt = ('dma', self.uid), 0
            else:
                self.uid += 1
                w0.dsem, w0.dcnt = ('dma', self.uid), 0
        w0.dcnt += 16
        key = w0.dsem
        val = w0.dcnt
        assert val < 60000, w0.name
        self.dma_tot[key] = val

        def fn(e, out=out, in_=in_, kw=kw):
            return e.dma_start(out=out, in_=in_, **kw)
        self.q[eng].append((wl, fn, (self.sem(key), 16)))
        for r in reads:
            if r.rd.get(key, 0) < val:
                r.rd[key] = val
        for w in writes:
            if w.wr.get(key, 0) < val:
                w.wr[key] = val
        if is_output:
            self.out_events[key] = val

    def barrier(self):
        evs = {}
        for e in ENGS:
            n = self.cnt[e]
            if n:
                evs[(e, (n - 1) // SEG)] = (n - 1) % SEG + 1
        evs.update(self.dma_tot)
        for e in ENGS:
            wl = []
            wd = self.waited[e]
            for k, v in evs.items():
                if e == 'pe' and k[0] == 'pe':
                    continue
                if wd.get(k, 0) < v:
                    wd[k] = v
                    wl.append((self.sem(k), v))
            if wl:
                self.q[e].append((wl, None, None))

    def finish(self):
        nc = self.nc
        self.barrier()
        q = self.q
        with nc.Block() as block:
            def replay(name):
                def run(eng):
                    for waits, fn, inc in q[name]:
                        for s, v in waits:
                            eng.wait_ge(s, v)
                        if fn is not None:
                            ins = fn(eng)
                            if inc is not None:
                                ins.then_inc(inc[0], inc[1])
                return run
            block.tensor(replay('pe'))
            block.scalar(replay('act'))
            block.vector(replay('dve'))
            block.gpsimd(replay('pool'))
            block.sync(replay('sp'))
        for ctx in reversed(self.semctx):
            ctx.__exit__(None, None, None)

    def mm(self, out, lhsT, rhs, start, stop, R, W):
        self.op('pe', lambda e: e.matmul(out, lhsT=lhsT, rhs=rhs, start=start, stop=stop), R, W)

    def tr(self, out, in_, ident, R, W):
        self.op('pe', lambda e: e.transpose(out, in_, ident), R, W)

    def act(self, out, in_, func, R, W, **kw):
        self.op('act', lambda e: e.activation(out=out, in_=in_, func=func, **kw), R, W)

    def cp(self, eng, out, in_, R, W, partial=False):
        if eng == 'act':
            self.op('act', lambda e: e.copy(out=out, in_=in_), R, W, partial)
        else:
            self.op(eng, lambda e: e.tensor_copy(out=out, in_=in_), R, W, partial)

    def tt(self, eng, out, in0, in1, op, R, W):
        self.op(eng, lambda e: e.tensor_tensor(out=out, in0=in0, in1=in1, op=op), R, W)

    def ts(self, eng, out, in0, s1, s2, op0, op1, R, W, accum_out=None):
        if accum_out is not None:
            self.op(eng, lambda e: e.tensor_scalar(out=out, in0=in0, scalar1=s1, scalar2=s2, op0=op0, op1=op1,
                                                   accum_out=accum_out), R, W)
        elif op1 is None:
            self.op(eng, lambda e: e.tensor_scalar(out=out, in0=in0, scalar1=s1, scalar2=None, op0=op0), R, W)
        else:
            self.op(eng, lambda e: e.tensor_scalar(out=out, in0=in0, scalar1=s1, scalar2=s2, op0=op0, op1=op1), R, W)

    def stt(self, out, in0, scalar, in1, op0, op1, R, W):
        self.op('dve', lambda e: e.scalar_tensor_tensor(out=out, in0=in0, scalar=scalar, in1=in1, op0=op0, op1=op1),
                R, W)

    def memset(self, eng, ap, val, W):
        self.op(eng, lambda e: e.memset(ap, val), (), W)


IN_SIZES = (256, 256, 256, 256, 128, 32, 256, 64, 64, 64, 64, 64, 64, 12, 256, 64, 64, 256, 32, 8)
IN_OFF = np.concatenate([[0], np.cumsum(IN_SIZES)]).astype(int)
(I_MQ, I_MK, I_MV, I_CQ, I_CKV, I_KR, I_NQ, I_NKC, I_NVC, I_NKS, I_NVS, I_NKW, I_NVW, I_NG,
 I_DQ, I_DK, I_DV, I_DIQ, I_DIK, I_DIW) = range(20)


def _cols(i):
    return np.arange(IN_OFF[i], IN_OFF[i + 1])


def _swap(c, half):
    c = np.asarray(c).reshape(-1, 2, half)
    return c[:, ::-1, :].reshape(-1)


def win_layout():
    segs = {}
    idx = []

    def add(name, cols):
        segs[name] = (len(idx), len(cols))
        idx.extend(list(cols))

    def addr(nm, i):
        c = _cols(i)
        add(nm, c)
        add(nm + '_s', _swap(c, 32))
    segs['MOBA0'] = (len(idx), 0)
    addr('mq', I_MQ)
    addr('mk', I_MK)
    add('mv', _cols(I_MV))
    segs['MOBA1'] = (len(idx), 0)
    segs['MLA0'] = (len(idx), 0)
    add('cq', _cols(I_CQ))
    add('ckv', _cols(I_CKV))
    kr = _cols(I_KR)
    add('kr96', np.concatenate([_cols(I_CKV)[:64], kr]))
    add('kr96_s', np.concatenate([_cols(I_CKV)[:64], _swap(kr, 16)]))
    segs['MLA1'] = (len(idx), 0)
    segs['NSA0'] = (len(idx), 0)
    addr('nq', I_NQ)
    addr('nkc', I_NKC)
    addr('nks', I_NKS)
    addr('nkw', I_NKW)
    add('nvc', _cols(I_NVC))
    add('nvs', _cols(I_NVS))
    add('nvw', _cols(I_NVW))
    add('ngrep', np.repeat(_cols(I_NG), 64))
    segs['NSA1'] = (len(idx), 0)
    segs['DSA0'] = (len(idx), 0)
    addr('dq', I_DQ)
    addr('dk', I_DK)
    add('dv', _cols(I_DV))
    iq = _cols(I_DIQ)
    iqc = []
    for ch in range(3):
        for k in range(4):
            hh = 3 * ch + k
            iqc.append(iq[hh * 32:(hh + 1) * 32] if (k < 3 and hh < 8) else iq[0:32])
    add('diq', np.concatenate(iqc))
    add('dik4', np.tile(_cols(I_DIK), 4))
    add('diw', _cols(I_DIW))
    segs['DSA1'] = (len(idx), 0)
    return np.asarray(idx, dtype=np.int64), segs


WIN_IDX, WSEG = win_layout()
NWIN = len(WIN_IDX)


def rope_tabs():
    def tabs(dim):
        inv = 1.0 / (10000.0 ** (np.arange(0, dim, 2, dtype=np.float32) / dim))
        ang = np.arange(T, dtype=np.float32)[:, None] * inv[None, :].astype(np.float32)
        return np.cos(ang).astype(np.float32), np.sin(ang).astype(np.float32)
    c64, s64 = tabs(64)
    c32, s32 = tabs(32)
    C = np.zeros((128, T), np.float32)
    Sg = np.zeros((128, T), np.float32)
    for b in (0, 64):
        C[b:b + 32] = c64.T
        C[b + 32:b + 64] = c64.T
        Sg[b:b + 32] = -s64.T
        Sg[b + 32:b + 64] = s64.T
    Cm = np.ones((128, T), np.float32)
    Sm = np.zeros((128, T), np.float32)
    Cm[64:80] = c32.T
    Cm[80:96] = c32.T
    Sm[64:80] = -s32.T
    Sm[80:96] = s32.T
    return C, Sg, Cm, Sm


def const_tables():
    import ml_dtypes
    bf = ml_dtypes.bfloat16
    k = np.arange(128)[:, None]
    q = np.arange(512)[None, :]
    mdiag = np.stack([((i * 128 + k) <= q) for i in range(4)]).astype(np.float32)
    mwin = np.stack([((r * 128 + k) > (q - 512)) for r in (-4, -3, -2, -1)]).astype(np.float32)
    masks = np.concatenate([mdiag, mwin], 0).transpose(1, 0, 2).astype(bf)
    key = np.arange(T)[None, :]
    e16 = (key // 256 == np.arange(16)[:, None]).astype(np.float32)
    e64 = (key // 64 == np.arange(64)[:, None]).astype(np.float32)
    eind = np.zeros((128, 2, T), np.float32)
    eind[64:80, 0] = e16
    eind[64:128, 1] = e64
    eind = eind.astype(bf)
    n = np.arange(256)[:, None]
    mc = ((16 * n + 31) <= np.arange(T)[None, :]) & (n < 255)
    mcmp = mc.reshape(2, 128, T).transpose(1, 0, 2).astype(bf)
    cs = np.arange(255) * 16
    ss = np.arange(64) * 64
    ov = ((cs[:, None] < ss[None, :] + 64) & (cs[:, None] + 32 > ss[None, :])).astype(np.float32)
    c2s = np.zeros((256, 65), np.float32)
    c2s[:255, :64] = ov
    c2s[:255, 64] = 1.0
    c2s = c2s.reshape(2, 128, 65).transpose(1, 0, 2).astype(bf)
    tq = np.arange(T)[:, None]
    sid = np.arange(64)[None, :]
    own = tq // 64
    causal = sid <= own
    forced = causal & ((sid == 0) | (sid >= own - 1))
    A = (causal & ~forced).astype(np.float32)
    Bm = np.where(forced, 1e4, np.where(causal, 0.0, -1e4)).astype(np.float32)
    A = A.reshape(32, 128, 64).transpose(1, 0, 2).copy()
    Bm = Bm.reshape(32, 128, 64).transpose(1, 0, 2).copy()
    qq = np.arange(128)[:, None]
    kk = np.arange(128)[None, :]
    dbias = np.where(kk <= qq, 0.0, -1e30).astype(np.float32)
    d01 = (kk <= qq).astype(np.float32).astype(bf)
    ident = np.eye(128, dtype=np.float32)
    return dict(masks=masks, eind=eind, mcmp=mcmp, c2s=c2s, nsaA=A, nsaB=Bm, dbias=dbias, d01=d01,
                ident=ident, identb=ident.astype(bf))


def pcol(v, n):
    return np.ascontiguousarray(np.asarray(v, np.float32).reshape(n, 128).T)


class K:
    pass


def build(nlayers=2, dbg=(), stop=None):
    nc = bass.Bass("TRN2", target_bir_lowering=False)
    S = Sched(nc)
    g = K()
    g.nc, g.S, g.dbg, g.stop = nc, S, set(dbg), stop
    din = {}

    def inp(name, shape, dt=F32):
        din[name] = nc.dram_tensor(name, list(shape), dt, kind="ExternalInput").ap()
        return din[name]
    g.din = din
    L = 2
    inp('x', [T, D])
    inp('cT', [128, 8])
    inp('ada_w', [L, D, 9 * D])
    inp('ada_bT', [L, 128, 72])
    inp('normsT', [L, 128, 3, 8])
    inp('fnorm_b', [128, D])
    for nm in ('ffn1', 'ffn2'):
        inp(nm + '_w_gate', [L, D, DFF])
        inp(nm + '_w_up', [L, D, DFF])
        inp(nm + '_w_down', [L, DFF, D])
    inp('w_in_g', [L, D, NWIN])
    inp('w_out', [L, D, D])
    inp('gnT', [L, 128, 8])
    inp('mla_qnT', [L, 128, 2])
    inp('mla_kvnT', [L, 128, 1])
    inp('mla_w_uq', [L, 256, 384])
    inp('mla_w_uq_s', [L, 256, 384])
    inp('mla_w_uk', [L, 128, 256])
    inp('mla_w_uv', [L, 128, 256])
    inp('nsa_peT', [L, 64, 2, 32])
    inp('nsa_w1', [L, 2, 2048, 256])
    inp('nsa_w2', [L, 2, 256, 64])
    inp('ropeC', [128, T])
    inp('ropeS', [128, T])
    inp('ropeCm', [128, T])
    inp('ropeSm', [128, T])
    inp('masks', [128, 8, 512], BF16)
    inp('eind', [128, 2, T], BF16)
    inp('mcmp', [128, 2, T], BF16)
    inp('c2s', [128, 2, 65], BF16)
    inp('nsaA', [128, 32, 64])
    inp('nsaB', [128, 32, 64])
    inp('dbias', [128, 128])
    inp('d01', [128, 128], BF16)
    inp('ident', [128, 128])
    inp('identb', [128, 128], BF16)

    def scratch(name, shape, dt):
        kind = "ExternalOutput" if name in g.dbg else "Internal"
        return nc.dram_tensor(name, list(shape), dt, kind=kind).ap()
    g.out = nc.dram_tensor("out", [T, D], F32, kind="ExternalOutput").ap()
    g.XT = scratch('XT', [8, 128, T], F32)
    g.HT = scratch('HT', [8, 128, T], BF16)
    g.OT = scratch('OT', [8, 128, T], F32)
    g.XTr = [Res('XT%d' % i) for i in range(NCH)]
    g.HTr = [Res('HT%d' % i) for i in range(NCH)]
    g.OTr = [[Res('OT%d_%d' % (gi, i)) for i in range(NCH)] for gi in range(4)]
    g.OUTr = [Res('out%d' % i) for i in range(NCH)]
    g.noR = []

    g.ps = [TT(nc.alloc_psum_tensor("psb%d" % i, [128, 512], F32), "psb%d" % i) for i in range(7)]
    for b_ in g.ps:
        b_.r[0].psum = True
    g.psi = 0
    g.dsa_nc = globals().get('DSA_NC', 1)
    g.pso = 0

    g.ident = S.tile([128, 128], F32, 'ident')
    g.identb = S.tile([128, 128], BF16, 'identb')
    g.onesb = S.tile([128, 128], BF16, 'onesb')
    g.sel64 = S.tile([128, 64], F32, 'sel64')
    g.ada = [S.tile([128, 72], F32, 'ada%d' % l) for l in range(L)]
    g.norms = [S.tile([128, 3, 8], F32, 'norms%d' % l) for l in range(L)]
    g.modAs = [[S.tile([128, 8], F32, 'modA%d_%d' % (l, i)) for i in range(3)] for l in range(L)]
    g.modGs = [[S.tile([128, 8], F32, 'modG%d_%d' % (l, i)) for i in range(3)] for l in range(L)]
    g.norm_done = set()
    S.dma('sp', g.ident[:], din['ident'], writes=g.ident.r)
    S.dma('sp', g.identb[:], din['identb'], writes=g.identb.r)
    S.memset('pool', g.onesb[:], 1.0, g.onesb.r)
    S.memset('pool', g.sel64[:], 0.0, g.sel64.r)
    S.memset('pool', g.sel64[64:65, :], 1.0, g.sel64.r)
    for l in range(L):
        S.dma('sp', g.norms[l][:], din['normsT'][l], writes=g.norms[l].r)

    prologue(g)
    for l in range(nlayers):
        adaln(g, l)
        for sub in range(3):
            set_mod(g, l, sub)
    if stop == 'pro':
        if 'ADA0' in g.dbg:
            dd = nc.dram_tensor('ADA0', [128, 72], F32, kind='ExternalOutput').ap()
            S.dma('sp', dd, g.ada[0][:], reads=g.ada[0].r, writes=[Res('ada0dbg')])
        S.finish()
        return nc
    for l in range(nlayers):
        if 'skipffn' not in g.dbg:
            ffn(g, l, 0, next_norm=(l, 1))
        if stop in ('ffn1', 'norm'):
            break
        mixer(g, l)
        if stop in ('mix', 'moba', 'mla', 'nsa', 'dsa'):
            break
        ffn(g, l, 2, next_norm=((l + 1, 0) if l + 1 < nlayers else None))
    if stop is None:
        epilogue(g)
    S.finish()
    print("[build] instr counts", S.cnt, "sems", len(S.sems), "sbuf peak", S.sb_peak)
    return nc


def dump(g, name, ap, shape, R, dt=F32):
    if name in g.dbg:
        dd = g.nc.dram_tensor(name, list(shape), dt, kind='ExternalOutput').ap()
        g.S.dma('sp', dd, ap, reads=R, writes=[Res(name + 'dbg')])


def psum(g):
    b = g.ps[g.psi % 5]
    g.psi += 1
    return b


def psum_o(g):
    b = g.ps[5 + g.pso % 2]
    g.pso += 1
    return b


def prologue(g):
    S, din = g.S, g.din
    m = S.mark()
    xs = [S.tile([128, D], F32, 'pxs%d' % i) for i in range(2)]
    xc = [S.tile([128, 8, 512], F32, 'pxc%d' % i) for i in range(2)]
    for tc in range(NCH):
        xct = xc[tc % 2]
        for j in range(4):
            tt_ = tc * 4 + j
            xt = xs[tt_ % 2]
            S.dma('sp', xt[:], din['x'][tt_ * 128:(tt_ + 1) * 128, :], writes=xt.r)
            for hb in range(2):
                pb = psum(g)
                for dd in range(4):
                    dc = hb * 4 + dd
                    S.tr(pb[:, dd * 128:(dd + 1) * 128], xt[:, dc * 128:(dc + 1) * 128], g.ident[:],
                         [xt.r[0], g.ident.r[0]], pb.r)
                eng = 'act' if hb == 0 else 'dve'
                S.cp(eng, xct[:, hb * 4:(hb + 1) * 4, j * 128:(j + 1) * 128],
                     pb[:].rearrange("p (a b) -> p a b", a=4), pb.r, xct.r, partial=True)
        S.dma('act', g.XT[:, :, tc * 512:(tc + 1) * 512].rearrange("c p t -> p c t"), xct[:],
              reads=xct.r, writes=[g.XTr[tc]], partial=False)
    S.release(m)


def eng_is_act(e):
    return hasattr(e, 'activation')


def adaln(g, l):
    S, din = g.S, g.din
    m = S.mark()
    cT = S.tile([128, 8], F32, 'cT')
    cact = S.tile([128, 8], F32, 'cact')
    arow = S.tile([1, 9 * D], F32, 'arow')
    one1 = S.tile([1, 1], F32, 'one1')
    bT = S.tile([128, 72], F32, 'adab')
    S.dma('sp', cT[:], din['cT'], writes=cT.r)
    S.dma('sp', bT[:], din['ada_bT'][l], writes=bT.r)
    S.memset('pool', one1[:], 1.0, one1.r)
    S.act(cact[:], cT[:], AF.Silu, cT.r, cact.r)
    NB = 1152
    wb = [S.tile([128, 8, NB], F32, 'adaw%d' % i) for i in range(2)]
    for blk in range(8):
        w = wb[blk % 2]
        S.dma('sp' if blk % 2 == 0 else 'act', w[:],
              din['ada_w'][l][:, blk * NB:(blk + 1) * NB].rearrange("(c p) n -> p c n", p=128),
              writes=w.r, partial=False)
        for sub in range(3):
            n0 = sub * 384
            pb = psum(g)
            for dc in range(8):
                S.mm(pb[0:1, 0:384], cact[:, dc:dc + 1], w[:, dc, n0:n0 + 384], dc == 0, dc == 7,
                     [cact.r[0], w.r[0]], pb.r)
            S.cp('act', arow[0:1, blk * NB + n0: blk * NB + n0 + 384], pb[0:1, 0:384], pb.r, arow.r)
    dump(g, 'AROW%d' % l, arow[:], [1, 9 * D], arow.r)
    dump(g, 'CACT%d' % l, cact[:], [128, 8], cact.r)
    pb = psum(g)
    for j in range(72):
        S.mm(pb[:, j:j + 1], arow[0:1, j * 128:(j + 1) * 128], one1[0:1, 0:1], True, True,
             [arow.r[0], one1.r[0]], pb.r)
    S.tt('dve', g.ada[l][:], pb[:, 0:72], bT[:], ALU.add, [pb.r[0], bT.r[0]], g.ada[l].r)
    S.release(m)


def set_mod(g, l, sub):
    S = g.S
    ada = g.ada[l]
    sc = ada[:, (3 * sub + 1) * 8:(3 * sub + 1) * 8 + 8]
    gt = ada[:, (3 * sub + 2) * 8:(3 * sub + 2) * 8 + 8]
    A, G = g.modAs[l][sub], g.modGs[l][sub]
    S.stt(A[:], sc, 1.0, g.norms[l][:, sub, :], ALU.add, ALU.mult, [ada.r[0], g.norms[l].r[0]], A.r)
    S.ts('dve', G[:], gt, 1.0 if sub == 1 else 0.5, None, ALU.mult, None, ada.r, G.r)


def mod_shift(g, l, sub):
    return g.ada[l][:, (3 * sub) * 8:(3 * sub) * 8 + 8]


class NormTmp:
    def __init__(self, g, nm):
        S = g.S
        self.sq = S.tile([128, 8, 512], BF16, nm + 'sq')
        self.rs = S.tile([128, 512], F32, nm + 'rs')
        self.tmp = [S.tile([128, 512], F32, nm + 'tmp%d' % i) for i in range(2)]


def norm_chunk(g, l, sub, x, h, nt):
    S = g.S
    A = g.modAs[l][sub]
    sh = mod_shift(g, l, sub)
    ada = g.ada[l]
    s, r = nt.sq, nt.rs
    S.act(s[:], x[:], AF.Square, x.r, s.r)
    pb = psum(g)
    for dc in range(8):
        S.mm(pb[:], g.onesb[:], s[:, dc, :], dc == 0, dc == 7, [g.onesb.r[0], s.r[0]], pb.r)
    S.act(r[:], pb[:], AF.Sqrt, pb.r, r.r, scale=1.0 / D, bias=1e-6)
    S.op('dve', lambda e, o=r[:]: e.reciprocal(out=o, in_=o), r.r, r.r)
    for dc in range(8):
        t_ = nt.tmp[dc % 2]
        S.stt(t_[:], x[:, dc, :], A[:, dc:dc + 1], r[:], ALU.mult, ALU.mult, [x.r[0], A.r[0], r.r[0]], t_.r)
        S.act(h[:, dc, :], t_[:], AF.Identity, [t_.r[0], ada.r[0]], h.r, bias=sh[:, dc:dc + 1], scale=1.0)


def norm_pass(g, l, sub):
    S = g.S
    if (l, sub) in g.norm_done:
        return
    g.norm_done.add((l, sub))
    m = S.mark()
    xs = [S.tile([128, 8, 512], F32, 'nxs%d' % i) for i in range(2)]
    hb = [S.tile([128, 8, 512], BF16, 'nhb%d' % i) for i in range(2)]
    nt = NormTmp(g, 'np')

    def nload(tc):
        S.dma('sp', xs[tc % 2][:], g.XT[:, :, tc * 512:(tc + 1) * 512].rearrange("c p t -> p c t"),
              reads=[g.XTr[tc]], writes=xs[tc % 2].r, partial=False)
    nload(0)
    for tc in range(NCH):
        x, h = xs[tc % 2], hb[tc % 2]
        if tc + 1 < NCH:
            nload(tc + 1)
        norm_chunk(g, l, sub, x, h, nt)
        S.dma('sp', g.HT[:, :, tc * 512:(tc + 1) * 512].rearrange("c p t -> p c t"), h[:],
              reads=h.r, writes=[g.HTr[tc]], partial=False)
    S.release(m)


def ffn(g, l, sub, next_norm=None):
    S, din = g.S, g.din
    nm = 'ffn1' if sub == 0 else 'ffn2'
    norm_pass(g, l, sub)
    modG = g.modGs[l][sub]
    if g.stop == 'norm':
        return
    m = S.mark()
    FH = DFF // 2
    wgs = [S.tile([128, 8, FH], BF16, 'wg0')] * 2
    wus = [S.tile([128, 8, FH], BF16, 'wu0')] * 2
    wds = [S.tile([128, 11, D], BF16, 'wd0')] * 2
    if next_norm is not None:
        nt = NormTmp(g, 'fn')
        hN = [S.tile([128, 8, 512], BF16, 'fhN%d' % i) for i in range(2)]
        g.norm_done.add(next_norm)
    hbs = [S.tile([128, 8, 512], BF16, 'fhb%d' % i) for i in range(2)]
    at = [S.tile([128, 11, 512], BF16, 'fat%d' % i) for i in range(2)]
    sg = [S.tile([128, 512], BF16, 'fsg%d' % i) for i in range(2)]
    xs = [S.tile([128, 8, 512], F32, 'fxs%d' % i) for i in range(2)]
    it = 0

    def fload(i_, tc_):
        S.dma('sp', hbs[i_ % 2][:], g.HT[:, :, tc_ * 512:(tc_ + 1) * 512].rearrange("c p t -> p c t"),
              reads=[g.HTr[tc_]], writes=hbs[i_ % 2].r, partial=False)
        S.dma('sp', xs[i_ % 2][:], g.XT[:, :, tc_ * 512:(tc_ + 1) * 512].rearrange("c p t -> p c t"),
              reads=[g.XTr[tc_]], writes=xs[i_ % 2].r, partial=False)
    fload(0, 0)
    for half in range(2):
        f0 = half * FH
        wg, wu, wd = wgs[half], wus[half], wds[half]
        for dc in range(8):
            S.dma('pool', wg[:, dc, :], din[nm + '_w_gate'][l][dc * 128:(dc + 1) * 128, f0:f0 + FH],
                  writes=wg.r, partial=(dc > 0))
            S.dma('pool', wu[:, dc, :], din[nm + '_w_up'][l][dc * 128:(dc + 1) * 128, f0:f0 + FH],
                  writes=wu.r, partial=(dc > 0))
        for fc in range(11):
            S.dma('pool', wd[:, fc, :], din[nm + '_w_down'][l][f0 + fc * 128:f0 + (fc + 1) * 128, :],
                  writes=wd.r, partial=(fc > 0))
        for tc in range(NCH):
            hb, a, x = hbs[it % 2], at[it % 2], xs[it % 2]
            it += 1
            if it < 2 * NCH:
                fload(it, it % NCH)
            for fc in range(11):
                pg, pu = psum(g), psum(g)
                for dc in range(8):
                    S.mm(pg[:], wg[:, dc, fc * 128:(fc + 1) * 128], hb[:, dc, :], dc == 0, dc == 7,
                         [wg.r[0], hb.r[0]], pg.r)
                for dc in range(8):
                    S.mm(pu[:], wu[:, dc, fc * 128:(fc + 1) * 128], hb[:, dc, :], dc == 0, dc == 7,
                         [wu.r[0], hb.r[0]], pu.r)
                s_ = sg[fc % 2]
                S.act(s_[:], pg[:], AF.Silu, pg.r, s_.r)
                S.tt('dve', a[:, fc, :], s_[:], pu[:], ALU.mult, [s_.r[0], pu.r[0]], a.r, )
            for dc in range(8):
                py = psum(g)
                for fc in range(11):
                    S.mm(py[:], wd[:, fc, dc * 128:(dc + 1) * 128], a[:, fc, :], fc == 0, fc == 10,
                         [wd.r[0], a.r[0]], py.r)
                S.stt(x[:, dc, :], py[:], modG[:, dc:dc + 1], x[:, dc, :], ALU.mult, ALU.add,
                      [py.r[0], modG.r[0], x.r[0]], x.r)
            S.dma('sp', g.XT[:, :, tc * 512:(tc + 1) * 512].rearrange("c p t -> p c t"), x[:],
                  reads=x.r, writes=[g.XTr[tc]], partial=False)
            if half == 1 and next_norm is not None:
                h_ = hN[tc % 2]
                norm_chunk(g, next_norm[0], next_norm[1], x, h_, nt)
                S.dma('sp', g.HT[:, :, tc * 512:(tc + 1) * 512].rearrange("c p t -> p c t"), h_[:],
                      reads=h_.r, writes=[g.HTr[tc]], partial=False)
    S.release(m)


def wseg(name):
    return WSEG[name][0]


def load_win(g, l, mix, name='win'):
    S, din = g.S, g.din
    c0, c1 = WSEG[mix + '0'][0], WSEG[mix + '1'][0]
    n = c1 - c0
    w = S.tile([128, 8, n], BF16, name)
    for dc in range(8):
        S.dma('pool', w[:, dc, :], din['w_in_g'][l][dc * 128:(dc + 1) * 128, c0:c1], writes=w.r, partial=(dc > 0))
    return w, c0


def proj(g, pb, w, col, M, hb, rows0=0):
    S = g.S
    for dc in range(8):
        S.mm(pb[0:M, :], w[:, dc, col:col + M], hb[:, dc, :], dc == 0, dc == 7, [w.r[0], hb.r[0]], pb.r)


def load_chunk_inputs(g, tc, hb, tabs):
    S, din = g.S, g.din
    S.dma('sp', hb[:], g.HT[:, :, tc * 512:(tc + 1) * 512].rearrange("c p t -> p c t"),
          reads=[g.HTr[tc]], writes=hb.r, partial=False)
    for t_, nm in tabs:
        S.dma('sp', t_[:], din[nm][:, tc * 512:(tc + 1) * 512], writes=t_.r, partial=False)


def rope_to(g, dst_ap, dstR, pa, pbs, Ct, St, r0, r1, tmp1, tmp2, extra=None):
    S = g.S
    S.tt('dve', tmp1[r0:r1, :], pa[r0:r1, :], Ct[r0:r1, :], ALU.mult, [pa.r[0], Ct.r[0]], tmp1.r)
    S.tt('dve', tmp2[r0:r1, :], pbs[r0:r1, :], St[r0:r1, :], ALU.mult, [pbs.r[0], St.r[0]], tmp2.r)
    if extra is not None:
        S.tt('pool', extra[0], tmp1[r0:r1, :], tmp2[r0:r1, :], ALU.add, [tmp1.r[0], tmp2.r[0]], extra[1])
        S.cp('act', dst_ap, extra[0], extra[1], dstR, partial=True)
    else:
        S.tt('pool', dst_ap, tmp1[r0:r1, :], tmp2[r0:r1, :], ALU.add, [tmp1.r[0], tmp2.r[0]], dstR)


class AttnWork:
    def __init__(self, g, nm):
        S = g.S
        self.pt = [S.tile([128, 512], BF16, nm + 'pt%d' % i) for i in range(5)]
        self.osb = [S.tile([128, 512], F32, nm + 'osb%d' % i) for i in range(2)]
        self.o = [S.tile([64, 512], F32, nm + 'o%d' % i) for i in range(2)]
        self.ip = 0
        self.io = 0


def attn_chunk(g, wk, c, klist, qfn, kfn, vfn, scale, Kc, skew=3, mask_engs=('pool',)):
    S = g.S
    po = psum_o(g)
    n = len(klist)
    pts = {}

    def stage1(i):
        kt, lo, mask, mR, mlo, mhi = klist[i]
        ps = psum(g)
        qa, qR = qfn(lo)
        ka, kR = kfn(kt)
        S.mm(ps[:, lo:512], ka, qa, True, True, qR + kR, ps.r)
        pt = wk.pt[wk.ip % 5]
        wk.ip += 1
        S.act(pt[:, lo:512], ps[:, lo:512], AF.Exp, ps.r, pt.r, scale=scale)
        if mask is not None:
            S.tt(mask_engs[i % len(mask_engs)], pt[:, mlo:mhi], pt[:, mlo:mhi], mask, ALU.mult, pt.r + mR, pt.r)
        pts[i] = pt

    def stage2(i):
        kt, lo, mask, mR, mlo, mhi = klist[i]
        va, vR = vfn(kt)
        pt = pts.pop(i)
        S.mm(po[0:65, lo:512], va, pt[:, lo:512], i == 0, i == n - 1, vR + pt.r, po.r)

    for i in range(min(skew, n)):
        stage1(i)
    for i in range(n):
        if i + skew < n:
            stage1(i + skew)
        stage2(i)
    return po


def normalize_o(g, wk, po):
    S = g.S
    osb = wk.osb[wk.io % 2]
    o = wk.o[wk.io % 2]
    wk.io += 1
    S.cp('act', osb[0:65, :], po[0:65, :], po.r, osb.r)
    S.ts('dve', osb[64:65, :], osb[64:65, :], 1e-30, None, ALU.max, None, osb.r, osb.r)
    S.op('dve', lambda e, a=osb[64:65, :]: e.reciprocal(out=a, in_=a), osb.r, osb.r)
    pb = psum(g)
    S.mm(pb[0:64, :], g.sel64[0:65, 0:64], osb[0:65, :], True, True, [g.sel64.r[0], osb.r[0]], pb.r)
    S.tt('dve', o[0:64, :], osb[0:64, :], pb[0:64, :], ALU.mult, [osb.r[0], pb.r[0]], o.r)
    return o


def store_o(g, o, grp, h, c):
    S = g.S
    ch = 2 * grp + h // 2
    p0 = (h % 2) * 64
    S.dma('sp', g.OT[ch, p0:p0 + 64, c * 512:(c + 1) * 512], o[0:64, :], reads=o.r, writes=[g.OTr[grp][c]])


def causal_klist(g, c, masks):
    kl = []
    for kt in range(4 * c + 4):
        i = kt - 4 * c
        if i < 0:
            kl.append((kt, 0, None, [], 0, 0))
        else:
            kl.append((kt, i * 128, masks[:, i, i * 128:(i + 1) * 128], [masks.r[0]], i * 128, (i + 1) * 128))
    return kl


def moba(g, l):
    S, din = g.S, g.din
    m = S.mark()
    w, c0 = load_win(g, l, 'MOBA')
    col = lambda nm: WSEG[nm][0] - c0
    masks = S.tile([128, 8, 512], BF16, 'masks')
    S.dma('sp', masks[:], din['masks'], writes=masks.r)
    QT = [S.tile([128, T], BF16, 'mQT%d' % h) for h in range(4)]
    KT = [S.tile([128, T], BF16, 'mKT%d' % h) for h in range(4)]
    VA = S.tile([128, 32, 4, 65], BF16, 'mVA')
    ksum = S.tile([64, 4, 16], F32, 'mksum')
    kmT = S.tile([64, 4, 16], BF16, 'mkmT')
    S.memset('pool', VA[:, :, :, 64:65], 1.0, VA.r)
    for h in range(4):
        S.dma('sp', KT[h][64:80, :], din['eind'][64:80, 0, :], writes=KT[h].r)
    m2 = S.mark()
    hbs = [S.tile([128, 8, 512], BF16, 'mhb%d' % i) for i in range(2)]
    Cs = [S.tile([128, 512], F32, 'mC%d' % i) for i in range(2)]
    Ss = [S.tile([128, 512], F32, 'mS%d' % i) for i in range(2)]
    t1 = [S.tile([64, 512], F32, 'mt1%d' % i) for i in range(2)]
    t2 = [S.tile([64, 512], F32, 'mt2%d' % i) for i in range(2)]
    kf = [S.tile([64, 512], F32, 'mkf%d' % i) for i in range(2)]
    it = 0
    for tc in range(NCH):
        hb, Ct, St = hbs[tc % 2], Cs[tc % 2], Ss[tc % 2]
        load_chunk_inputs(g, tc, hb, [(Ct, 'ropeC'), (St, 'ropeS')])
        sl = slice(tc * 512, (tc + 1) * 512)
        for h in range(4):
            pa, pb_ = psum(g), psum(g)
            proj(g, pa, w, col('mq') + h * 64, 64, hb)
            proj(g, pb_, w, col('mq_s') + h * 64, 64, hb)
            rope_to(g, QT[h][0:64, sl], QT[h].r, pa, pb_, Ct, St, 0, 64, t1[it % 2], t2[it % 2])
            it += 1
            pa, pb_ = psum(g), psum(g)
            proj(g, pa, w, col('mk') + h * 64, 64, hb)
            proj(g, pb_, w, col('mk_s') + h * 64, 64, hb)
            kf_ = kf[it % 2]
            rope_to(g, KT[h][0:64, sl], KT[h].r, pa, pb_, Ct, St, 0, 64, t1[it % 2], t2[it % 2],
                    extra=(kf_[0:64, :], kf_.r))
            S.op('dve', lambda e, o=ksum[:, h, 2 * tc:2 * tc + 2], i=kf_[0:64, :].rearrange("p (a b) -> p a b", a=2):
                 e.tensor_reduce(out=o, in_=i, axis=AX.X, op=ALU.add), kf_.r, ksum.r, partial=True)
            it += 1
        for j in range(4):
            pv = psum(g)
            for dc in range(8):
                S.mm(pv[:, 0:256], hb[:, dc, j * 128:(j + 1) * 128], w[:, dc, col('mv'):col('mv') + 256],
                     dc == 0, dc == 7, [hb.r[0], w.r[0]], pv.r)
            S.cp('act', VA[:, tc * 4 + j, :, 0:64], pv[:, 0:256].rearrange("p (a b) -> p a b", a=4), pv.r, VA.r,
                 partial=True)
    S.release(m2)
    S.act(kmT[:], ksum[:], AF.Copy, ksum.r, kmT.r, scale=1.0 / 256)
    G = [S.tile([128, 16], F32, 'mG%d' % i) for i in range(2)]
    m8 = [S.tile([128, 8], F32, 'mm8%d' % i) for i in range(2)]
    NBT = [S.tile([128, 128], BF16, 'mNBT%d' % i) for i in range(4)]
    for t_ in NBT:
        S.memset('pool', t_[:], 0.0, t_.r)
    it = 0
    for h in range(4):
        for c in range(NCH):
            pt_ = psum(g)
            for j in range(4):
                qt = 4 * c + j
                own = qt // 2
                nb = NBT[it % 4]
                if own >= 4:
                    G_, m8_ = G[it % 2], m8[it % 2]
                    pg = psum(g)
                    S.mm(pg[:, 0:16], QT[h][0:64, qt * 128:(qt + 1) * 128], kmT[0:64, h, :], True, True,
                         [QT[h].r[0], kmT.r[0]], pg.r)
                    S.memset('pool', G_[:], -1e30, G_.r)
                    S.cp('dve', G_[:, 0:own], pg[:, 0:own], pg.r, G_.r)
                    S.op('dve', lambda e, o=m8_[:], i=G_[:]: e.max(out=o, in_=i), G_.r, m8_.r)
                    S.ts('dve', nb[:, 64:80], G_[:], m8_[:, 2:3], NEGB, ALU.is_lt, ALU.mult,
                         [G_.r[0], m8_.r[0]], nb.r)
                    S.memset('pool', nb[:, 64 + own:65 + own], 0.0, nb.r)
                else:
                    S.memset('pool', nb[:, 64:80], NEGB, nb.r)
                    S.memset('pool', nb[:, 64:65 + own], 0.0, nb.r)
                it += 1
                S.mm(pt_[0:80, j * 128:(j + 1) * 128], nb[:, 0:80], g.identb[:], True, True,
                     [nb.r[0], g.identb.r[0]], pt_.r)
            S.cp('act', QT[h][64:80, c * 512:(c + 1) * 512], pt_[64:80, :], pt_.r, QT[h].r, partial=True)
    wk = AttnWork(g, 'm')
    for h in range(4):
        for c in range(NCH):
            kl = causal_klist(g, c, masks)
            po = attn_chunk(g, wk, c, kl,
                            lambda lo, h=h, c=c: (QT[h][0:80, c * 512 + lo:(c + 1) * 512], [QT[h].r[0]]),
                            lambda kt, h=h: (KT[h][0:80, kt * 128:(kt + 1) * 128], [KT[h].r[0]]),
                            lambda kt, h=h: (VA[:, kt, h, :], [VA.r[0]]),
                            0.125, 80)
            o = normalize_o(g, wk, po)
            store_o(g, o, 0, h, c)
    S.release(m)


def rms_feat(g, srcs, nrm, gains, outs, sqs, rtile, width):
    S = g.S
    n = len(srcs)
    for i in range(n):
        S.act(sqs[i][:], srcs[i][:], AF.Square, srcs[i].r, sqs[i].r)
    pss = psum(g)
    for i in range(n):
        S.mm(pss[:], g.onesb[:], sqs[i][:], i == 0, i == n - 1, [g.onesb.r[0], sqs[i].r[0]], pss.r)
    S.act(rtile[:], pss[:], AF.Sqrt, pss.r, rtile.r, scale=1.0 / width, bias=1e-6)
    S.op('dve', lambda e, o=rtile[:]: e.reciprocal(out=o, in_=o), rtile.r, rtile.r)
    for i in range(n):
        S.stt(outs[i][:], srcs[i][:], gains[:, i:i + 1], rtile[:], ALU.mult, ALU.mult,
              [srcs[i].r[0], gains.r[0], rtile.r[0]], outs[i].r)


def mla(g, l):
    S, din = g.S, g.din
    m = S.mark()
    w, c0 = load_win(g, l, 'MLA')
    col = lambda nm: WSEG[nm][0] - c0
    masks = S.tile([128, 8, 512], BF16, 'masks')
    S.dma('sp', masks[:], din['masks'], writes=masks.r)
    wuq = S.tile([128, 2, 384], BF16, 'wuq')
    wuqs = S.tile([128, 2, 384], BF16, 'wuqs')
    wuk = S.tile([128, 256], BF16, 'wuk')
    wuv = S.tile([128, 256], BF16, 'wuv')
    qn = S.tile([128, 2], F32, 'qn')
    kvn = S.tile([128, 1], F32, 'kvn')
    S.dma('pool', wuq[:], din['mla_w_uq'][l].rearrange("(c p) n -> p c n", p=128), writes=wuq.r)
    S.dma('pool', wuqs[:], din['mla_w_uq_s'][l].rearrange("(c p) n -> p c n", p=128), writes=wuqs.r)
    S.dma('pool', wuk[:], din['mla_w_uk'][l], writes=wuk.r)
    S.dma('pool', wuv[:], din['mla_w_uv'][l], writes=wuv.r)
    S.dma('sp', qn[:], din['mla_qnT'][l], writes=qn.r)
    S.dma('sp', kvn[:], din['mla_kvnT'][l], writes=kvn.r)
    QT = [S.tile([128, T], BF16, 'aQT%d' % h) for h in range(4)]
    KT = [S.tile([128, T], BF16, 'aKT%d' % h) for h in range(4)]
    VA = S.tile([128, 32, 4, 65], BF16, 'aVA')
    S.memset('pool', VA[:, :, :, 64:65], 1.0, VA.r)
    m2 = S.mark()
    hbs = [S.tile([128, 8, 512], BF16, 'ahb%d' % i) for i in range(2)]
    Cs = [S.tile([128, 512], F32, 'aC%d' % i) for i in range(2)]
    Ss = [S.tile([128, 512], F32, 'aS%d' % i) for i in range(2)]
    sq = [S.tile([128, 512], BF16, 'asq%d' % i) for i in range(3)]
    cqn = [S.tile([128, 512], BF16, 'acqn%d' % i) for i in range(2)]
    ckvn = S.tile([128, 512], BF16, 'ackvn')
    rq = S.tile([128, 512], F32, 'arq')
    rkv = S.tile([128, 512], F32, 'arkv')
    t1 = [S.tile([128, 512], F32, 'at1%d' % i) for i in range(2)]
    t2 = [S.tile([128, 512], F32, 'at2%d' % i) for i in range(2)]
    kpe = S.tile([128, 512], BF16, 'akpe')
    it = 0
    for tc in range(NCH):
        hb, Ct, St = hbs[tc % 2], Cs[tc % 2], Ss[tc % 2]
        load_chunk_inputs(g, tc, hb, [(Ct, 'ropeCm'), (St, 'ropeSm')])
        sl = slice(tc * 512, (tc + 1) * 512)
        pc = [psum(g), psum(g)]
        for i in range(2):
            proj(g, pc[i], w, col('cq') + i * 128, 128, hb)
        rms_feat(g, pc, None, qn, cqn, sq[0:2], rq, 256.0)
        pk = psum(g)
        proj(g, pk, w, col('ckv'), 128, hb)
        rms_feat(g, [pk], None, kvn, [ckvn], sq[2:3], rkv, 128.0)
        for h in range(4):
            pa, pb_ = psum(g), psum(g)
            for i in range(2):
                S.mm(pa[0:96, :], wuq[:, i, h * 96:(h + 1) * 96], cqn[i][:], i == 0, i == 1,
                     [wuq.r[0], cqn[i].r[0]], pa.r)
            for i in range(2):
                S.mm(pb_[0:96, :], wuqs[:, i, h * 96:(h + 1) * 96], cqn[i][:], i == 0, i == 1,
                     [wuqs.r[0], cqn[i].r[0]], pb_.r)
            S.cp('dve', QT[h][0:64, sl], pa[0:64, :], pa.r, QT[h].r, partial=True)
            rope_to(g, QT[h][64:96, sl], QT[h].r, pa, pb_, Ct, St, 64, 96, t1[it % 2], t2[it % 2])
            it += 1
            pkn = psum(g)
            S.mm(pkn[0:64, :], wuk[:, h * 64:(h + 1) * 64], ckvn[:], True, True, [wuk.r[0], ckvn.r[0]], pkn.r)
            S.cp('act', KT[h][0:64, sl], pkn[0:64, :], pkn.r, KT[h].r, partial=True)
        pa, pb_ = psum(g), psum(g)
        proj(g, pa, w, col('kr96'), 96, hb)
        proj(g, pb_, w, col('kr96_s'), 96, hb)
        rope_to(g, kpe[64:96, :], kpe.r, pa, pb_, Ct, St, 64, 96, t1[it % 2], t2[it % 2])
        it += 1
        for h in range(4):
            S.cp('pool', KT[h][64:96, sl], kpe[64:96, :], kpe.r, KT[h].r, partial=True)
        for j in range(4):
            pv = psum(g)
            S.mm(pv[:, 0:256], ckvn[:, j * 128:(j + 1) * 128], wuv[:], True, True, [ckvn.r[0], wuv.r[0]], pv.r)
            S.cp('act', VA[:, tc * 4 + j, :, 0:64], pv[:, 0:256].rearrange("p (a b) -> p a b", a=4), pv.r, VA.r,
                 partial=True)
    S.release(m2)
    wk = AttnWork(g, 'a')
    sc = float(96 ** -0.5)
    for h in range(4):
        for c in range(NCH):
            kl = causal_klist(g, c, masks)
            po = attn_chunk(g, wk, c, kl,
                            lambda lo, h=h, c=c: (QT[h][0:96, c * 512 + lo:(c + 1) * 512], [QT[h].r[0]]),
                            lambda kt, h=h: (KT[h][0:96, kt * 128:(kt + 1) * 128], [KT[h].r[0]]),
                            lambda kt, h=h: (VA[:, kt, h, :], [VA.r[0]]),
                            sc, 96)
            o = normalize_o(g, wk, po)
            store_o(g, o, 1, h, c)
    S.release(m)


NIT = 14


def dsa(g, l):
    S, din = g.S, g.din
    m = S.mark()
    dbias = S.tile([128, 128], F32, 'dbias')
    S.dma('sp', dbias[:], din['dbias'], writes=dbias.r)
    QT = [S.tile([64, T], BF16, 'dQT%d' % h) for h in range(4)]
    KT = S.tile([64, T], BF16, 'dKT')
    VA = S.tile([128, 32, 65], BF16, 'dVA')
    IQ = S.tile([128, 3, T], BF16, 'dIQ')
    IK = S.tile([128, T], BF16, 'dIK')
    IW = S.tile([128, 32, 8], F32, 'dIW')
    S.memset('pool', VA[:, :, 64:65], 1.0, VA.r)
    m2 = S.mark()
    w, c0 = load_win(g, l, 'DSA')
    col = lambda nm: WSEG[nm][0] - c0
    hbs = [S.tile([128, 8, 512], BF16, 'dhb%d' % i) for i in range(2)]
    Cs = [S.tile([128, 512], F32, 'dC%d' % i) for i in range(2)]
    Ss = [S.tile([128, 512], F32, 'dS%d' % i) for i in range(2)]
    t1 = [S.tile([64, 512], F32, 'dt1%d' % i) for i in range(2)]
    t2 = [S.tile([64, 512], F32, 'dt2%d' % i) for i in range(2)]
    it = 0
    for tc in range(NCH):
        hb, Ct, St = hbs[tc % 2], Cs[tc % 2], Ss[tc % 2]
        load_chunk_inputs(g, tc, hb, [(Ct, 'ropeC'), (St, 'ropeS')])
        sl = slice(tc * 512, (tc + 1) * 512)
        for h in range(5):
            pa, pb_ = psum(g), psum(g)
            if h < 4:
                proj(g, pa, w, col('dq') + h * 64, 64, hb)
                proj(g, pb_, w, col('dq_s') + h * 64, 64, hb)
                dst = QT[h]
            else:
                proj(g, pa, w, col('dk'), 64, hb)
                proj(g, pb_, w, col('dk_s'), 64, hb)
                dst = KT
            rope_to(g, dst[0:64, sl], dst.r, pa, pb_, Ct, St, 0, 64, t1[it % 2], t2[it % 2])
            it += 1
        for i in range(3):
            pa = psum(g)
            proj(g, pa, w, col('diq') + i * 128, 128, hb)
            S.cp('act', IQ[:, i, sl], pa[:], pa.r, IQ.r, partial=True)
        pa = psum(g)
        proj(g, pa, w, col('dik4'), 128, hb)
        S.cp('act', IK[:, sl], pa[:], pa.r, IK.r, partial=True)
        for j in range(4):
            pv = psum(g)
            for dc in range(8):
                S.mm(pv[:, 0:64], hb[:, dc, j * 128:(j + 1) * 128], w[:, dc, col('dv'):col('dv') + 64],
                     dc == 0, dc == 7, [hb.r[0], w.r[0]], pv.r)
            S.cp('act', VA[:, tc * 4 + j, 0:64], pv[:, 0:64], pv.r, VA.r, partial=True)
            pw = psum(g)
            for dc in range(8):
                S.mm(pw[:, 0:8], hb[:, dc, j * 128:(j + 1) * 128], w[:, dc, col('diw'):col('diw') + 8],
                     dc == 0, dc == 7, [hb.r[0], w.r[0]], pw.r)
            S.cp('dve', IW[:, tc * 4 + j, :], pw[:, 0:8], pw.r, IW.r, partial=True)
    S.release(m2)
    if 'dsa_stop_proj' in g.dbg:
        S.release(m)
        return
    acc = [S.tile([128, T], F32, 'dacc%d' % i) for i in range(2)]
    rl = [S.tile([128, 512], BF16, 'drl%d' % i) for i in range(4)]
    Dg = [S.tile([128, 8, 128], BF16, 'dDg%d' % i) for i in range(2)]
    Mq = [S.tile([128, T], BF16, 'dM%d' % i) for i in range(4)]
    MT = S.tile([128, 32, 512], BF16, 'dMT')
    X = [S.tile([128, 1], F32, 'dX%d' % i) for i in range(2)]
    STEP = [S.tile([128, NIT], F32, 'dSTEP%d' % i) for i in range(2)]
    CK = S.tile([128, NIT], F32, 'dCK')
    thr = [S.tile([128, 1], F32, 'dthr%d' % i) for i in range(2)]
    cand = [S.tile([128, 1], F32, 'dcand%d' % i) for i in range(2)]
    cnt = [S.tile([128, 1], F32, 'dcnt%d' % i) for i in range(2)]
    gs = [S.tile([128, 1], F32, 'dgs%d' % i) for i in range(2)]
    for k in range(NIT):
        S.memset('pool', CK[:, k:k + 1], float(2.0 ** (-k)), CK.r)
    wk = AttnWork(g, 'd')
    irl = 0
    for c in range(NCH if 'dsa_c1' not in g.dbg else int(g.dsa_nc)):
        for jp in range(2):
            js = (2 * jp, 2 * jp + 1)
            for j in js:
                qt = 4 * c + j
                nk = (qt + 1) * 128
                a_, D_ = acc[j % 2], Dg[j % 2]
                for hh in range(8):
                    S.ts('pool', D_[:, hh, :], g.identb[:], IW[:, qt, hh:hh + 1], None, ALU.mult, None,
                         [g.identb.r[0], IW.r[0]], D_.r)
                for kc in range((nk + 511) // 512):
                    cols = min(512, nk - kc * 512)
                    ks = slice(kc * 512, kc * 512 + cols)
                    pacc = psum_o(g)
                    rls = {}

                    def st1(hh):
                        pb = 32 * (hh % 3)
                        ps = psum(g)
                        S.mm(ps[:, 0:cols], IQ[pb:pb + 32, hh // 3, qt * 128:(qt + 1) * 128], IK[pb:pb + 32, ks],
                             True, True, [IQ.r[0], IK.r[0]], ps.r)
                        r_ = rl[st1.i % 4]
                        st1.i += 1
                        if hh % 2 == 0:
                            S.act(r_[:, 0:cols], ps[:, 0:cols], AF.Relu, ps.r, r_.r)
                        else:
                            S.ts('dve', r_[:, 0:cols], ps[:, 0:cols], 0.0, None, ALU.max, None, ps.r, r_.r)
                        rls[hh] = r_
                    st1.i = irl

                    def st2(hh):
                        r_ = rls.pop(hh)
                        S.mm(pacc[:, 0:cols], D_[:, hh, :], r_[:, 0:cols], hh == 0, hh == 7, [D_.r[0], r_.r[0]],
                             pacc.r)
                    st1(0)
                    st1(1)
                    for hh in range(8):
                        if hh + 2 < 8:
                            st1(hh + 2)
                        st2(hh)
                    irl = st1.i
                    S.cp('dve', a_[:, ks], pacc[:, 0:cols], pacc.r, a_.r, partial=True)
                S.tt('dve', a_[:, qt * 128:nk], a_[:, qt * 128:nk], dbias[:], ALU.add, [a_.r[0], dbias.r[0]], a_.r)
            act_js = []
            for j in js:
                qt = 4 * c + j
                a_, X_, ST_, cd_ = acc[j % 2], X[j % 2], STEP[j % 2], cand[j % 2]
                if qt >= 2 and 'dsa_nobisect' not in g.dbg:
                    S.op('dve', lambda e, o=X_[:], i=a_[:, 0:qt * 128]: e.tensor_reduce(
                        out=o, in_=i, axis=AX.X, op=ALU.max, apply_absolute_value=True), a_.r, X_.r)
                    S.ts('dve', X_[:], X_[:], 1.001, 1e-6, ALU.mult, ALU.add, X_.r, X_.r)
                    chainB = len(act_js) == 1
                    S.ts('dve', ST_[:], CK[:], X_[:, 0:1], -1.0 if chainB else 1.0, ALU.mult, ALU.mult,
                         [CK.r[0], X_.r[0]], ST_.r)
                    S.memset('pool', cd_[:], 0.0, cd_.r)
                    act_js.append(j)
            for k in range(NIT):
                for ci, j in enumerate(act_js):
                    qt = 4 * c + j
                    nk = (qt + 1) * 128
                    a_, ST_, cd_, cn_, gs_, Mj = (acc[j % 2], STEP[j % 2], cand[j % 2], cnt[j % 2], gs[j % 2], Mq[j])
                    if ci == 0:
                        S.ts('dve', Mj[:, 0:nk], a_[:, 0:nk], cd_[:, 0:1], None, ALU.is_ge, ALU.add,
                             [a_.r[0], cd_.r[0]], [Mj.r[0], cn_.r[0]], accum_out=cn_[:])
                        S.ts('dve', gs_[:], cn_[:], 255.5, 0.5, ALU.is_ge, ALU.subtract, cn_.r, gs_.r)
                        S.stt(cd_[:], gs_[:], ST_[:, k:k + 1], cd_[:], ALU.mult, ALU.add,
                              [gs_.r[0], ST_.r[0], cd_.r[0]], cd_.r)
                    else:
                        S.act(Mj[:, 0:nk], a_[:, 0:nk], AF.Sign, [a_.r[0], cd_.r[0]], [Mj.r[0], cn_.r[0]],
                              bias=cd_[:, 0:1], scale=1.0, accum_out=cn_[:])
                        S.ts('pool', gs_[:], cn_[:], float(511 - nk), 0.5, ALU.is_ge, ALU.subtract, cn_.r, gs_.r)
                        S.ts('pool', cd_[:], gs_[:], ST_[:, k:k + 1], cd_[:, 0:1], ALU.mult, ALU.add,
                             [gs_.r[0], ST_.r[0], cd_.r[0]], cd_.r)
            for ci, j in enumerate(act_js):
                X_, cd_, th_ = X[j % 2], cand[j % 2], thr[j % 2]
                if ci == 1:
                    S.ts('dve', cd_[:], cd_[:], -1.0, None, ALU.mult, None, cd_.r, cd_.r)
                S.stt(th_[:], X_[:], float(-(2.0 ** (-NIT))), cd_[:], ALU.mult, ALU.add, [X_.r[0], cd_.r[0]], th_.r)
            for j in js:
                qt = 4 * c + j
                nk = (qt + 1) * 128
                a_, th_, Mj = acc[j % 2], thr[j % 2], Mq[j]
                if j in act_js:
                    S.ts('dve', Mj[:, 0:nk], a_[:, 0:nk], th_[:, 0:1], None, ALU.is_ge, None, [a_.r[0], th_.r[0]],
                         Mj.r)
                else:
                    S.ts('dve', Mj[:, 0:nk], a_[:, 0:nk], -1e29, None, ALU.is_ge, None, a_.r, Mj.r)
        for kt in range(4 * c + 4):
            i0 = max(0, kt - 4 * c)
            pt_ = psum(g)
            for j in range(i0, 4):
                S.mm(pt_[:, j * 128:(j + 1) * 128], Mq[j][:, kt * 128:(kt + 1) * 128], g.identb[:], True, True,
                     [Mq[j].r[0], g.identb.r[0]], pt_.r)
            S.cp('act', MT[:, kt, i0 * 128:512], pt_[:, i0 * 128:512], pt_.r, MT.r, partial=True)
        for h in range(4 if 'dsa_noattn' not in g.dbg else 0):
            kl = []
            for kt in range(4 * c + 4):
                lo = max(0, kt - 4 * c) * 128
                kl.append((kt, lo, MT[:, kt, lo:512], [MT.r[0]], lo, 512))
            po = attn_chunk(g, wk, c, kl,
                            lambda lo, h=h, c=c: (QT[h][0:64, c * 512 + lo:(c + 1) * 512], [QT[h].r[0]]),
                            lambda kt: (KT[0:64, kt * 128:(kt + 1) * 128], [KT.r[0]]),
                            lambda kt: (VA[:, kt, :], [VA.r[0]]),
                            0.125, 64, mask_engs=('pool', 'dve'))
            o = normalize_o(g, wk, po)
            store_o(g, o, 3, h, c)
    S.release(m)


def nsa(g, l):
    S, din = g.S, g.din
    m = S.mark()
    w, c0 = load_win(g, l, 'NSA')
    col = lambda nm: WSEG[nm][0] - c0
    masks = S.tile([128, 8, 512], BF16, 'masks')
    S.dma('sp', masks[:], din['masks'], writes=masks.r)
    QT = [S.tile([128, T], BF16, 'nQT%d' % h) for h in range(4)]
    KS = S.tile([128, T], BF16, 'nKS')
    KW = S.tile([64, T], BF16, 'nKW')
    VAs = S.tile([128, 32, 65], BF16, 'nVAs')
    VAw = S.tile([128, 32, 65], BF16, 'nVAw')
    KcmpT = S.tile([64, 256], BF16, 'nKcmp')
    VAc = S.tile([128, 2, 65], BF16, 'nVAc')
    c2s = S.tile([128, 2, 65], BF16, 'nc2s')
    S.dma('sp', c2s[:], din['c2s'], writes=c2s.r)
    S.dma('sp', KS[64:128, :], din['eind'][64:128, 1, :], writes=KS.r)
    S.memset('pool', VAs[:, :, 64:65], 1.0, VAs.r)
    S.memset('pool', VAw[:, :, 64:65], 1.0, VAw.r)
    S.memset('pool', VAc[:], 0.0, VAc.r)
    S.memset('pool', VAc[:, :, 64:65], 1.0, VAc.r)
    S.memset('pool', KcmpT[:], 0.0, KcmpT.r)
    m2 = S.mark()
    KC = S.tile([64, T], F32, 'nKC')
    VC = S.tile([64, T], F32, 'nVC')
    m3 = S.mark()
    hbs = [S.tile([128, 8, 512], BF16, 'nhb%d' % i) for i in range(2)]
    Cs = [S.tile([128, 512], F32, 'nC%d' % i) for i in range(2)]
    Ss = [S.tile([128, 512], F32, 'nS%d' % i) for i in range(2)]
    t1 = [S.tile([64, 512], F32, 'nt1%d' % i) for i in range(2)]
    t2 = [S.tile([64, 512], F32, 'nt2%d' % i) for i in range(2)]
    it = 0
    for tc in range(NCH if 'nsa_noloop' not in g.dbg else 0):
        hb, Ct, St = hbs[tc % 2], Cs[tc % 2], Ss[tc % 2]
        load_chunk_inputs(g, tc, hb, [(Ct, 'ropeC'), (St, 'ropeS')])
        sl = slice(tc * 512, (tc + 1) * 512)
        for h in range(7):
            pa, pb_ = psum(g), psum(g)
            if h < 4:
                nm, off, dst = 'nq', h * 64, QT[h]
            else:
                nm, off, dst = (('nkc', 0, KC), ('nks', 0, KS), ('nkw', 0, KW))[h - 4]
            proj(g, pa, w, col(nm) + off, 64, hb)
            proj(g, pb_, w, col(nm + '_s') + off, 64, hb)
            rope_to(g, dst[0:64, sl], dst.r, pa, pb_, Ct, St, 0, 64, t1[it % 2], t2[it % 2])
            it += 1
        pa = psum(g)
        proj(g, pa, w, col('nvc'), 64, hb)
        S.cp('act', VC[0:64, sl], pa[0:64, :], pa.r, VC.r, partial=True)
        for j in range(4):
            pv = psum(g)
            for dc in range(8):
                S.mm(pv[:, 0:128], hb[:, dc, j * 128:(j + 1) * 128], w[:, dc, col('nvs'):col('nvs') + 128],
                     dc == 0, dc == 7, [hb.r[0], w.r[0]], pv.r)
            S.cp('act', VAs[:, tc * 4 + j, 0:64], pv[:, 0:64], pv.r, VAs.r, partial=True)
            S.cp('act', VAw[:, tc * 4 + j, 0:64], pv[:, 64:128], pv.r, VAw.r, partial=True)
    S.release(m3)
    if 'nsa_stop1' in g.dbg:
        S.release(m)
        return
    peT = S.tile([64, 2, 32], F32, 'npe')
    S.dma('sp', peT[:], din['nsa_peT'][l], writes=peT.r)
    w1 = S.tile([64, 32, 256], BF16, 'nw1')
    w2 = S.tile([128, 2, 64], BF16, 'nw2')
    Xall = S.tile([64, 32, 256], BF16, 'nXall')
    hid = [S.tile([128, 256], BF16, 'nhid%d' % i) for i in range(2)]
    S.memset('pool', Xall[:], 0.0, Xall.r)
    for x in range(2):
        src = KC if x == 0 else VC
        S.dma('pool', w1[:], din['nsa_w1'][l, x].rearrange("(j d) m -> d j m", d=64), writes=w1.r, partial=False)
        S.dma('pool', w2[:], din['nsa_w2'][l, x].rearrange("(c p) n -> p c n", p=128), writes=w2.r, partial=False)
        for j in range(32):
            S.ts('dve' if j % 2 == 0 else 'pool', Xall[:, j, 0:255], src[0:64, j:j + 16 * 254 + 1:16],
                 peT[:, x, j:j + 1], None, ALU.add, None, [src.r[0], peT.r[0]], Xall.r)
        for mc in range(2):
            ph = psum(g)
            for j in range(32):
                S.mm(ph[:, 0:256], w1[:, j, mc * 128:(mc + 1) * 128], Xall[:, j, :], j == 0, j == 31,
                     [w1.r[0], Xall.r[0]], ph.r)
            S.act(hid[mc][:], ph[:, 0:256], AF.Silu, ph.r, hid[mc].r)
        if x == 0:
            pk = psum(g)
            for mc in range(2):
                S.mm(pk[0:64, 0:256], w2[:, mc, :], hid[mc][:], mc == 0, mc == 1, [w2.r[0], hid[mc].r[0]], pk.r)
            S.cp('act', KcmpT[:, 0:255], pk[0:64, 0:255], pk.r, KcmpT.r)
        else:
            for nt in range(2):
                pv = psum(g)
                for mc in range(2):
                    S.mm(pv[:, 0:64], hid[mc][:, nt * 128:(nt + 1) * 128], w2[:, mc, :], mc == 0, mc == 1,
                         [hid[mc].r[0], w2.r[0]], pv.r)
                S.cp('act', VAc[:, nt, 0:64], pv[:, 0:64], pv.r, VAc.r)
    S.release(m2)
    if 'nsa_stop2' in g.dbg:
        S.release(m)
        return
    mcmp = S.tile([128, 2, T], BF16, 'nmcmp')
    S.dma('sp', mcmp[:], din['mcmp'], writes=mcmp.r)
    nA = S.tile([128, 32, 64], F32, 'nA')
    nB = S.tile([128, 32, 64], F32, 'nB')
    S.dma('sp', nA[:], din['nsaA'], writes=nA.r)
    S.dma('sp', nB[:], din['nsaB'], writes=nB.r)
    hbs = [S.tile([128, 8, 512], BF16, 'nhb2%d' % i) for i in range(1)]
    Gt = [S.tile([64, 12, 512], F32, 'nGt%d' % i) for i in range(1)]
    imp = [S.tile([128, 4, 64], F32, 'nimp%d' % i) for i in range(2)]
    rd = [S.tile([128, 1], F32, 'nrd%d' % i) for i in range(2)]
    acc = [S.tile([64, 512], F32, 'nacc%d' % i) for i in range(4)]
    m8a = [S.tile([128, 8], F32, 'nm8a%d' % i) for i in range(2)]
    m8b = [S.tile([128, 8], F32, 'nm8b%d' % i) for i in range(2)]
    imr = [S.tile([128, 64], F32, 'nimr%d' % i) for i in range(2)]
    NBT = [S.tile([128, 128], BF16, 'nNBT%d' % i) for i in range(4)]
    nbs = S.tile([128, 512], BF16, 'nnbs')
    for t_ in NBT:
        S.memset('pool', t_[:], 0.0, t_.r)
    wk = AttnWork(g, 'n')
    ird = 0
    load_chunk_inputs(g, 0, hbs[0], [])
    for c in range(NCH):
        hb, G_, imp_ = hbs[0], Gt[0], imp[c % 2]
        for gi in range(12):
            pa = psum(g)
            proj(g, pa, w, col('ngrep') + gi * 64, 64, hb)
            S.act(G_[:, gi, :], pa[0:64, :], AF.Sigmoid, pa.r, G_.r)
        if c + 1 < NCH:
            load_chunk_inputs(g, c + 1, hb, [])
        ntl = [0] if c <= 3 else [0, 1]
        for h in range(4):
            po = psum_o(g)
            pts = []
            for ii, nt in enumerate(ntl):
                ps = psum(g)
                S.mm(ps[:], KcmpT[0:64, nt * 128:(nt + 1) * 128], QT[h][0:64, c * 512:(c + 1) * 512], True, True,
                     [KcmpT.r[0], QT[h].r[0]], ps.r)
                pt = wk.pt[wk.ip % 5]
                wk.ip += 1
                S.act(pt[:], ps[:], AF.Exp, ps.r, pt.r, scale=0.125)
                S.tt('pool', pt[:], pt[:], mcmp[:, nt, c * 512:(c + 1) * 512], ALU.mult, [pt.r[0], mcmp.r[0]], pt.r)
                S.mm(po[0:65, :], VAc[:, nt, :], pt[:], ii == 0, ii == len(ntl) - 1, [VAc.r[0], pt.r[0]], po.r)
                pts.append(pt)
            for j in range(4):
                pi = psum(g)
                for ii, nt in enumerate(ntl):
                    S.mm(pi[:, 0:65], pts[ii][:, j * 128:(j + 1) * 128], c2s[:, nt, :], ii == 0, ii == len(ntl) - 1,
                         [pts[ii].r[0], c2s.r[0]], pi.r)
                r_ = rd[ird % 2]
                ird += 1
                S.ts('dve', r_[:], pi[:, 64:65], 1e-30, None, ALU.max, None, pi.r, r_.r)
                S.op('dve', lambda e, a=r_[:]: e.reciprocal(out=a, in_=a), r_.r, r_.r)
                if h == 0:
                    S.ts('dve', imp_[:, j, :], pi[:, 0:64], r_[:, 0:1], None, ALU.mult, None, [pi.r[0], r_.r[0]],
                         imp_.r)
                else:
                    S.stt(imp_[:, j, :], pi[:, 0:64], r_[:, 0:1], imp_[:, j, :], ALU.mult, ALU.add,
                          [pi.r[0], r_.r[0], imp_.r[0]], imp_.r)
            o = normalize_o(g, wk, po)
            S.tt('dve', acc[h][:], o[0:64, :], G_[:, h * 3 + 0, :], ALU.mult, [o.r[0], G_.r[0]], acc[h].r)
        pt_ = psum(g)
        for j in range(4):
            qt = 4 * c + j
            im, a8, b8 = imr[j % 2], m8a[j % 2], m8b[j % 2]
            nb = NBT[j]
            S.tt('dve', im[:], imp_[:, j, :], nA[:, qt, :], ALU.mult, [imp_.r[0], nA.r[0]], im.r)
            S.tt('dve', im[:], im[:], nB[:, qt, :], ALU.add, [im.r[0], nB.r[0]], im.r)
            S.op('dve', lambda e, o_=a8[:], i=im[:]: e.max(out=o_, in_=i), im.r, a8.r)
            S.op('dve', lambda e, o_=nb[:, 0:64], r=a8[:], v=im[:]: e.match_replace(out=o_, in_to_replace=r,
                                                                                  in_values=v, imm_value=-1e30),
                 [a8.r[0], im.r[0]], nb.r)
            S.op('dve', lambda e, o_=b8[:], i=nb[:, 0:64]: e.max(out=o_, in_=i), nb.r, b8.r)
            S.ts('dve', nb[:, 64:128], im[:], b8[:, 7:8], NEGB, ALU.is_lt, ALU.mult, [im.r[0], b8.r[0]], nb.r)
            S.memset('pool', nb[:, 0:64], 0.0, nb.r)
            S.mm(pt_[:, j * 128:(j + 1) * 128], nb[:], g.identb[:], True, True, [nb.r[0], g.identb.r[0]], pt_.r)
        S.cp('act', nbs[64:128, :], pt_[64:128, :], pt_.r, nbs.r)
        for h in range(4):
            S.cp('pool', QT[h][64:128, c * 512:(c + 1) * 512], nbs[64:128, :], nbs.r, QT[h].r, partial=True)
        for h in range(4):
            kl = causal_klist(g, c, masks)
            po = attn_chunk(g, wk, c, kl,
                            lambda lo, h=h, c=c: (QT[h][:, c * 512 + lo:(c + 1) * 512], [QT[h].r[0]]),
                            lambda kt: (KS[:, kt * 128:(kt + 1) * 128], [KS.r[0]]),
                            lambda kt: (VAs[:, kt, :], [VAs.r[0]]),
                            0.125, 128)
            o = normalize_o(g, wk, po)
            S.tt('dve', o[0:64, :], o[0:64, :], G_[:, h * 3 + 1, :], ALU.mult, [o.r[0], G_.r[0]], o.r)
            S.tt('pool', acc[h][:], acc[h][:], o[0:64, :], ALU.add, [acc[h].r[0], o.r[0]], acc[h].r)
            kl = []
            for kt in range(max(0, 4 * c - 4), 4 * c + 4):
                r = kt - 4 * c
                if r < 0:
                    kl.append((kt, 0, masks[:, 8 + r, :], [masks.r[0]], 0, 512))
                else:
                    kl.append((kt, r * 128, masks[:, r, r * 128:(r + 1) * 128], [masks.r[0]], r * 128, (r + 1) * 128))
            po = attn_chunk(g, wk, c, kl,
                            lambda lo, h=h, c=c: (QT[h][0:64, c * 512 + lo:(c + 1) * 512], [QT[h].r[0]]),
                            lambda kt: (KW[0:64, kt * 128:(kt + 1) * 128], [KW.r[0]]),
                            lambda kt: (VAw[:, kt, :], [VAw.r[0]]),
                            0.125, 64)
            o = normalize_o(g, wk, po)
            S.tt('dve', o[0:64, :], o[0:64, :], G_[:, h * 3 + 2, :], ALU.mult, [o.r[0], G_.r[0]], o.r)
            S.tt('pool', acc[h][:], acc[h][:], o[0:64, :], ALU.add, [acc[h].r[0], o.r[0]], acc[h].r)
            store_o(g, acc[h], 2, h, c)
    S.release(m)


def wout(g, l, next_norm=None):
    S, din = g.S, g.din
    m = S.mark()
    wo = S.tile([128, 8, D], BF16, 'wo')
    S.dma('pool', wo[:], din['w_out'][l].rearrange("(c p) n -> p c n", p=128), writes=wo.r)
    gn = S.tile([128, 8], F32, 'gn')
    S.dma('sp', gn[:], din['gnT'][l], writes=gn.r)
    ots = [S.tile([128, 8, 512], F32, 'wot%d' % i) for i in range(2)]
    xs = [S.tile([128, 8, 512], F32, 'wxs%d' % i) for i in range(2)]
    ys = [S.tile([128, 8, 512], BF16, 'wy%d' % i) for i in range(2)]
    sq = [S.tile([128, 2, 512], BF16, 'wsq%d' % i) for i in range(2)]
    rt = [S.tile([128, 512], F32, 'wrt%d' % i) for i in range(2)]
    modG = g.modGs[l][1]
    if next_norm is not None:
        nt = NormTmp(g, 'wn')
        hN = [S.tile([128, 8, 512], BF16, 'whN%d' % i) for i in range(2)]
        g.norm_done.add(next_norm)
    it = 0

    def wload(tc_):
        S.dma('sp', ots[tc_ % 2][:], g.OT[:, :, tc_ * 512:(tc_ + 1) * 512].rearrange("c p t -> p c t"),
              reads=[g.OTr[gi][tc_] for gi in range(4)], writes=ots[tc_ % 2].r, partial=False)
        S.dma('sp', xs[tc_ % 2][:], g.XT[:, :, tc_ * 512:(tc_ + 1) * 512].rearrange("c p t -> p c t"),
              reads=[g.XTr[tc_]], writes=xs[tc_ % 2].r, partial=False)
    wload(0)
    for tc in range(NCH):
        ot, x, y = ots[tc % 2], xs[tc % 2], ys[tc % 2]
        if tc + 1 < NCH:
            wload(tc + 1)
        for grp in range(4):
            s_, r_ = sq[it % 2], rt[it % 2]
            it += 1
            S.act(s_[:], ot[:, 2 * grp:2 * grp + 2, :], AF.Square, ot.r, s_.r)
            pss = psum(g)
            for i in range(2):
                S.mm(pss[:], g.onesb[:], s_[:, i, :], i == 0, i == 1, [g.onesb.r[0], s_.r[0]], pss.r)
            S.act(r_[:], pss[:], AF.Sqrt, pss.r, r_.r, scale=1.0 / 256, bias=1e-6)
            S.op('dve', lambda e, o=r_[:]: e.reciprocal(out=o, in_=o), r_.r, r_.r)
            for i in range(2):
                ch = 2 * grp + i
                S.stt(y[:, ch, :], ot[:, ch, :], gn[:, ch:ch + 1], r_[:], ALU.mult, ALU.mult,
                      [ot.r[0], gn.r[0], r_.r[0]], y.r)
        for dc in range(8):
            py = psum(g)
            for mc in range(8):
                S.mm(py[:], wo[:, mc, dc * 128:(dc + 1) * 128], y[:, mc, :], mc == 0, mc == 7,
                     [wo.r[0], y.r[0]], py.r)
            S.stt(x[:, dc, :], py[:], modG[:, dc:dc + 1], x[:, dc, :], ALU.mult, ALU.add,
                  [py.r[0], modG.r[0], x.r[0]], x.r)
        S.dma('sp', g.XT[:, :, tc * 512:(tc + 1) * 512].rearrange("c p t -> p c t"), x[:],
              reads=x.r, writes=[g.XTr[tc]], partial=False)
        if next_norm is not None:
            h_ = hN[tc % 2]
            norm_chunk(g, next_norm[0], next_norm[1], x, h_, nt)
            S.dma('sp', g.HT[:, :, tc * 512:(tc + 1) * 512].rearrange("c p t -> p c t"), h_[:],
                  reads=h_.r, writes=[g.HTr[tc]], partial=False)
    S.release(m)


def mixer(g, l):
    S = g.S
    norm_pass(g, l, 1)
    if 'skipmoba' not in g.dbg:
        moba(g, l)
    if g.stop == 'moba':
        return
    if 'skipmla' not in g.dbg:
        mla(g, l)
    if g.stop == 'mla':
        return
    if 'skipdsa' not in g.dbg:
        dsa(g, l)
    if g.stop == 'dsa':
        return
    if 'skipnsa' not in g.dbg:
        nsa(g, l)
    if g.stop == 'nsa':
        return
    wout(g, l, next_norm=(l, 2))


def epilogue(g):
    S, din = g.S, g.din
    m = S.mark()
    fn = S.tile([128, D], F32, 'fnb')
    S.dma('sp', fn[:], din['fnorm_b'], writes=fn.r)
    xs = [S.tile([128, 8, 512], F32, 'exs%d' % i) for i in range(2)]
    xt = [S.tile([128, D], F32, 'ext%d' % i) for i in range(2)]
    junk = S.tile([128, D], F32, 'ejunk')
    ss = [S.tile([128, 1], F32, 'ess%d' % i) for i in range(2)]
    it = 0

    def eload(tc_):
        S.dma('sp', xs[tc_ % 2][:], g.XT[:, :, tc_ * 512:(tc_ + 1) * 512].rearrange("c p t -> p c t"),
              reads=[g.XTr[tc_]], writes=xs[tc_ % 2].r, partial=False)
    eload(0)
    for tc in range(NCH):
        x = xs[tc % 2]
        if tc + 1 < NCH:
            eload(tc + 1)
        for j in range(4):
            o, s_ = xt[it % 2], ss[it % 2]
            it += 1
            for hb in range(2):
                pb = psum(g)
                for dd in range(4):
                    dc = hb * 4 + dd
                    S.tr(pb[:, dd * 128:(dd + 1) * 128], x[:, dc, j * 128:(j + 1) * 128], g.ident[:],
                         [x.r[0], g.ident.r[0]], pb.r)
                S.cp('act' if hb == 0 else 'dve', o[:, hb * 512:(hb + 1) * 512], pb[:], pb.r, o.r)
            S.act(junk[:], o[:], AF.Square, o.r, [junk.r[0], s_.r[0]], accum_out=s_[:])
            S.act(s_[:], s_[:], AF.Sqrt, s_.r, s_.r, scale=1.0 / D, bias=1e-6)
            S.op('dve', lambda e, a=s_[:]: e.reciprocal(out=a, in_=a), s_.r, s_.r)
            S.stt(o[:], o[:], s_[:, 0:1], fn[:], ALU.mult, ALU.mult, [o.r[0], s_.r[0], fn.r[0]], o.r)
            tt_ = tc * 4 + j
            S.dma('sp', g.out[tt_ * 128:(tt_ + 1) * 128, :], o[:], reads=o.r, writes=[g.OUTr[tc]], is_output=True)
    S.release(m)


_CACHE = {}


def prep_inputs(inp):
    f = lambda a: np.ascontiguousarray(np.asarray(a, np.float32))
    sh = {}
    L = 2
    sh['ada_w'] = f(inp['ada_w'])
    sh['ada_bT'] = np.stack([pcol(inp['ada_b'][l], 72) for l in range(L)])
    sh['normsT'] = np.stack([np.stack([pcol(inp[k][l], 8) for k in ('ffn1_norm', 'mix_norm', 'ffn2_norm')], 1)
                             for l in range(L)])
    sh['fnorm_b'] = np.ascontiguousarray(np.broadcast_to(f(inp['final_norm'])[None, :], (128, D)))
    for nm in ('ffn1', 'ffn2'):
        for w in ('_w_gate', '_w_up', '_w_down'):
            sh[nm + w] = f(inp[nm + w])
    sh['w_in_g'] = np.ascontiguousarray(f(inp['w_in'])[:, :, WIN_IDX])
    sh['w_out'] = f(inp['w_out'])
    sh['gnT'] = np.stack([pcol(np.asarray(inp['group_norm'][l]).reshape(-1), 8) for l in range(L)])
    sh['mla_qnT'] = np.stack([pcol(inp['mla_q_norm'][l], 2) for l in range(L)])
    sh['mla_kvnT'] = np.stack([pcol(inp['mla_kv_norm'][l], 1) for l in range(L)])
    uq = f(inp['mla_w_uq'])
    sh['mla_w_uq'] = uq
    sidx = np.arange(384).reshape(4, 96).copy()
    for h in range(4):
        sidx[h, 64:96] = _swap(sidx[h, 64:96], 16)
    sh['mla_w_uq_s'] = np.ascontiguousarray(uq[:, :, sidx.reshape(-1)])
    sh['mla_w_uk'] = f(inp['mla_w_uk'])
    sh['mla_w_uv'] = f(inp['mla_w_uv'])
    pe = np.stack([f(inp['nsa_pe_k']), f(inp['nsa_pe_v'])], 1)
    sh['nsa_peT'] = np.ascontiguousarray(pe.transpose(0, 3, 1, 2))
    sh['nsa_w1'] = np.stack([f(inp['nsa_cmp_k_w1']), f(inp['nsa_cmp_v_w1'])], 1)
    sh['nsa_w2'] = np.stack([f(inp['nsa_cmp_k_w2']), f(inp['nsa_cmp_v_w2'])], 1)
    C, Sg, Cm, Sm = rope_tabs()
    sh.update(ropeC=C, ropeS=Sg, ropeCm=Cm, ropeSm=Sm)
    sh.update(const_tables())
    x = f(inp['x'])
    c = f(inp['c'])
    maps = []
    for b in range(8):
        mp = dict(sh)
        mp['x'] = x[b]
        mp['cT'] = pcol(c[b], 8)
        maps.append(mp)
    return maps


def kernel(**inputs):
    maps = prep_inputs(inputs)
    if 'nc' not in _CACHE:
        _CACHE['nc'] = build()
    res = run_bass_kernel_spmd(_CACHE['nc'], maps, core_ids=list(range(8)))
    return np.stack([np.asarray(r['out'], np.float32) for r in res.results], 0)
```
